# Optimizing a Trainium2 kernel written in Bass

```python
import math
import jax
import jax.numpy as jnp
from jax import lax

D_MODEL = 1024
BATCH = 2
SEQ = 16384
DEPTH = 4

GRID_W = 64
CTX_LEN = 256
N_MIXERS = 4
RECURRENT_MIXERS = (0, 1, 2)
NORM_EPS = 1e-6
CHUNK = 128
CONV_SIZE = 3

SSM_D_INNER = 2 * D_MODEL
SSM_HEAD_DIM = 64
SSM_HEADS = SSM_D_INNER // SSM_HEAD_DIM
SSM_GROUPS = 8
SSM_STATE = 128
SSM_CONV_DIM = SSM_D_INNER + 2 * SSM_GROUPS * SSM_STATE
SSM_PROJ = SSM_D_INNER + SSM_CONV_DIM + 2 * SSM_HEADS

GDN_HEADS = 8
GDN_DK = 128
GDN_DV = 256
GDN_QK = GDN_HEADS * GDN_DK
GDN_VW = GDN_HEADS * GDN_DV
GDN_CONV_DIM = 2 * GDN_QK + GDN_VW
GDN_PROJ = GDN_CONV_DIM + GDN_VW + 4 * GDN_HEADS

RWKV_HEAD = 64
RWKV_HEADS = D_MODEL // RWKV_HEAD
RWKV_LORA_W = 64
RWKV_LORA_A = 64
RWKV_LORA_G = 160
RWKV_GN_EPS = 64e-5

FNET_GROUPS = 4

N_EXPERTS = 16
EC_CAPACITY = 2
EXPERT_FF = 1024

N_LAYERS_SSM = (DEPTH + 3) // 4
N_LAYERS_GDN = (DEPTH + 2) // 4
N_LAYERS_RWKV = (DEPTH + 1) // 4
N_LAYERS_FNET = DEPTH // 4

kernel_name = 'hybrid_diffusion_ssd_gdn_rwkv7_fnet_ecmoe'


def rmsnorm(x, w):
    xf = x.astype(jnp.float32)
    y = xf * lax.rsqrt(jnp.mean(xf * xf, axis=-1, keepdims=True) + NORM_EPS)
    return (y * w.astype(jnp.float32)).astype(x.dtype)


def l2norm(x):
    xf = x.astype(jnp.float32)
    return xf * lax.rsqrt(jnp.sum(xf * xf, axis=-1, keepdims=True) + NORM_EPS)


def modulate(h, shift, scale):
    return h * (1 + scale) + shift


def conv_latent(x, w):
    b, n, ch = x.shape
    rows = n // GRID_W
    y = lax.conv_general_dilated(x.reshape(b, rows, GRID_W, ch), w[:, :, None, :], (1, 1), 'SAME',
                                 dimension_numbers=('NHWC', 'HWIO', 'NHWC'), feature_group_count=ch)
    return y.reshape(b, n, ch)


def conv_context(x, w):
    ch = x.shape[-1]
    return lax.conv_general_dilated(x, w[CONV_SIZE // 2][:, None, :], (1,), 'SAME',
                                    dimension_numbers=('NWC', 'WIO', 'NWC'), feature_group_count=ch)


def centred_shift(h):
    hp = jnp.pad(h, ((0, 0), (1, 1), (0, 0)))
    return 0.5 * (hp[:, :-2] + hp[:, 2:])


def to_chunks(z, n_chunks):
    return jnp.moveaxis(z.reshape(z.shape[0], n_chunks, CHUNK, *z.shape[2:]), 1, 0)


def from_chunks(z):
    z = jnp.moveaxis(z, 0, 1)
    return z.reshape(z.shape[0], -1, *z.shape[3:])


def ssd_scan(xdt, la, bm, cm, s0=None):
    b, t, h, p = xdt.shape
    g, n = bm.shape[2], bm.shape[3]
    r = h // g
    nc = t // CHUNK
    f = jnp.float32
    xs = (to_chunks(xdt.astype(f).reshape(b, t, g, r, p), nc), to_chunks(la.astype(f).reshape(b, t, g, r), nc),
          to_chunks(bm.astype(f), nc), to_chunks(cm.astype(f), nc))
    if s0 is None:
        s0 = jnp.zeros((b, g, r, p, n), f)
    causal = jnp.tril(jnp.ones((CHUNK, CHUNK), bool))

    def step(s, inp):
        xc, lc, bc, cc = inp
        cs = jnp.cumsum(lc, axis=1)
        seg = cs[:, :, None] - cs[:, None, :]
        dec = jnp.exp(jnp.where(causal[None, :, :, None, None], seg, -jnp.inf))
        cb = jnp.einsum('blgn,bsgn->blsg', cc, bc)
        y = jnp.einsum('blsg,blsgr,bsgrp->blgrp', cb, dec, xc)
        y = y + jnp.einsum('blgn,bgrpn->blgrp', cc, s) * jnp.exp(cs)[..., None]
        tail = jnp.exp(cs[:, -1:] - cs)
        s = s * jnp.exp(cs[:, -1])[..., None, None] + jnp.einsum('blgr,blgrp,blgn->bgrpn', tail, xc, bc)
        return s, y

    s, ys = lax.scan(step, s0, xs)
    return from_chunks(ys).reshape(b, t, h, p), s


def gdn_scan(q, k, v, beta, g, s0=None):
    b, t, h, dk = q.shape
    dv = v.shape[-1]
    nc = t // CHUNK
    f = jnp.float32
    xs = tuple(to_chunks(z.astype(f), nc) for z in (q, k, v, beta, g))
    if s0 is None:
        s0 = jnp.zeros((b, h, dk, dv), f)
    incl = jnp.tril(jnp.ones((CHUNK, CHUNK), bool))
    strict = jnp.tril(jnp.ones((CHUNK, CHUNK), bool), -1)
    eye = jnp.eye(CHUNK, dtype=f)

    def step(s, inp):
        qc, kc, vc, bc, gc = inp
        qt, kt, vt = (z.transpose(0, 2, 1, 3) for z in (qc, kc, vc))
        bt = bc.transpose(0, 2, 1)[..., None]
        cs = jnp.cumsum(gc, axis=1).transpose(0, 2, 1)
        dmask = jnp.exp(jnp.where(incl, cs[..., :, None] - cs[..., None, :], -jnp.inf))
        kb = kt * bt
        a_mat = eye + jnp.where(strict, jnp.einsum('bhlk,bhsk->bhls', kb, kt) * dmask, 0.0)
        rhs = jnp.concatenate([vt * bt, kb * jnp.exp(cs)[..., None]], axis=-1)
        sol = lax.linalg.triangular_solve(a_mat, rhs, left_side=True, lower=True, unit_diagonal=True)
        u, wk = sol[..., :dv], sol[..., dv:]
        v_new = u - jnp.einsum('bhlk,bhkv->bhlv', wk, s)
        attn = jnp.einsum('bhlk,bhsk->bhls', qt, kt) * dmask
        o = jnp.einsum('bhlk,bhkv->bhlv', qt * jnp.exp(cs)[..., None], s) + jnp.einsum('bhls,bhsv->bhlv', attn, v_new)
        s = s * jnp.exp(cs[..., -1])[..., None, None] + jnp.einsum('bhlk,bhlv->bhkv', kt * jnp.exp(cs[..., -1:] - cs)[..., None], v_new)
        return s, o.transpose(0, 2, 1, 3)

    s, ys = lax.scan(step, s0, xs)
    return from_chunks(ys), s


def rwkv_scan(r, w, k, v, a, bv, s0=None):
    b, t, h, n = r.shape
    xs = tuple(jnp.moveaxis(z.astype(jnp.float32), 1, 0) for z in (r, w, k, v, a, bv))
    if s0 is None:
        s0 = jnp.zeros((b, h, n, n), jnp.float32)

    def step(s, inp):
        rt, wt, kt, vt, at, btt = inp
        sa = jnp.einsum('bhvk,bhk->bhv', s, at)
        s = s * wt[:, :, None, :] + sa[..., None] * btt[:, :, None, :] + vt[..., None] * kt[:, :, None, :]
        return s, jnp.einsum('bhvk,bhk->bhv', s, rt)

    s, ys = lax.scan(step, s0, xs, unroll=8)
    return jnp.moveaxis(ys, 0, 1), s


def flip_time(z, direction):
    return jnp.flip(z, axis=1) if direction == 1 else z


def bidirectional_scan(scan_fn, ctx_args, lat_args):
    y_ctx, y_lat = 0.0, 0.0
    for d in range(2):
        yc, s_ctx = scan_fn(*[flip_time(z, d) for z in ctx_args[d]])
        yl, _ = scan_fn(*[flip_time(z, d) for z in lat_args[d]], s0=s_ctx)
        y_ctx = y_ctx + flip_time(yc, d)
        y_lat = y_lat + flip_time(yl, d)
    return y_ctx, y_lat


def mamba2_mixer(hc, hl, in_w, conv_w, conv_b, a_log, dt_bias, d_skip, norm_w, out_w, need_ctx_out):
    def prep(h, conv):
        b, t, _ = h.shape
        z, xbc, dt = jnp.split(h @ in_w, [SSM_D_INNER, SSM_D_INNER + SSM_CONV_DIM], axis=-1)
        xbc = jax.nn.silu(conv(xbc, conv_w) + conv_b)
        xs, bm, cm = jnp.split(xbc, [SSM_D_INNER, SSM_D_INNER + SSM_GROUPS * SSM_STATE], axis=-1)
        xs = xs.reshape(b, t, SSM_HEADS, SSM_HEAD_DIM)
        bm = bm.reshape(b, t, SSM_GROUPS, SSM_STATE)
        cm = cm.reshape(b, t, SSM_GROUPS, SSM_STATE)
        dt = jax.nn.softplus(dt.reshape(b, t, 2, SSM_HEADS).astype(jnp.float32) + dt_bias)
        la = -jnp.exp(a_log.astype(jnp.float32)) * dt
        args = [(xs * dt[:, :, d, :, None], la[:, :, d], bm, cm) for d in range(2)]
        return z, xs, args

    zc, xc, ac = prep(hc, conv_context)
    zl, xl, al = prep(hl, conv_latent)
    yc, yl = bidirectional_scan(ssd_scan, ac, al)

    def finish(y, xs, z):
        b, t = z.shape[:2]
        y = y.astype(xs.dtype) + d_skip[:, None] * xs
        y = y.reshape(b, t, SSM_D_INNER) * jax.nn.silu(z)
        y = rmsnorm(y.reshape(b, t, SSM_GROUPS, -1), norm_w.reshape(SSM_GROUPS, -1)).reshape(b, t, SSM_D_INNER)
        return y @ out_w

    return (finish(yc, xc, zc) if need_ctx_out else None), finish(yl, xl, zl)


def gated_deltanet_mixer(hc, hl, in_w, conv_w, a_log, dt_bias, norm_w, out_w, need_ctx_out):
    def prep(h, conv):
        b, t, _ = h.shape
        qkv, z, ga, gb = jnp.split(h @ in_w, [GDN_CONV_DIM, GDN_CONV_DIM + GDN_VW, GDN_CONV_DIM + GDN_VW + 2 * GDN_HEADS], axis=-1)
        q, k, v = jnp.split(jax.nn.silu(conv(qkv, conv_w)), [GDN_QK, 2 * GDN_QK], axis=-1)
        q = l2norm(q.reshape(b, t, GDN_HEADS, GDN_DK)) * GDN_DK ** -0.5
        k = l2norm(k.reshape(b, t, GDN_HEADS, GDN_DK))
        v = v.reshape(b, t, GDN_HEADS, GDN_DV)
        g = -jnp.exp(a_log.astype(jnp.float32)) * jax.nn.softplus(ga.reshape(b, t, 2, GDN_HEADS).astype(jnp.float32) + dt_bias)
        beta = jax.nn.sigmoid(gb.reshape(b, t, 2, GDN_HEADS).astype(jnp.float32))
        return z, [(q, k, v, beta[:, :, d], g[:, :, d]) for d in range(2)]

    zc, ac = prep(hc, conv_context)
    zl, al = prep(hl, conv_latent)
    oc, ol = bidirectional_scan(gdn_scan, ac, al)

    def finish(o, z):
        b, t = z.shape[:2]
        o = rmsnorm(o.astype(z.dtype), norm_w) * jax.nn.silu(z.reshape(b, t, GDN_HEADS, GDN_DV))
        return o.reshape(b, t, GDN_VW) @ out_w

    return (finish(oc, zc) if need_ctx_out else None), finish(ol, zl)


def head_groupnorm(y, w, b):
    bsz, t, h, n = y.shape
    mu = jnp.mean(y, axis=-1, keepdims=True)
    var = jnp.mean(jnp.square(y - mu), axis=-1, keepdims=True)
    yn = ((y - mu) * lax.rsqrt(var + RWKV_GN_EPS)).reshape(bsz, t, h * n)
    return yn * w.astype(jnp.float32) + b.astype(jnp.float32)


def rwkv7_mixer(hc, hl, mu, w_r, w_k, w_v, w_o, w0, w1, w2, a0, a1, a2, g1, g2, k_k, k_a, r_k, ln_w, ln_b, need_ctx_out):
    def prep(h):
        b, t, _ = h.shape
        heads = lambda z: z.reshape(b, t, RWKV_HEADS, RWKV_HEAD)
        xx = centred_shift(h) - h
        xr, xw, xk, xv, xa, xg = (h + xx * mu[q] for q in range(6))
        r = heads(xr @ w_r)
        v = heads(xv @ w_v)
        k = xk @ w_k
        kk = l2norm(heads(k * k_k))
        args, k_dirs = [], []
        for d in range(2):
            w_log = -jax.nn.softplus(-(w0[d] + jnp.tanh(xw @ w1[d]) @ w2[d]).astype(jnp.float32)) - 0.5
            decay = heads(jnp.exp(-jnp.exp(w_log)))
            a_rate = heads(jax.nn.sigmoid((a0[d] + (xa @ a1[d]) @ a2[d]).astype(jnp.float32)))
            k_dir = heads(k) * (1 + (a_rate - 1) * k_a.reshape(RWKV_HEADS, RWKV_HEAD))
            args.append((r, decay, k_dir, v, -kk, kk * a_rate))
            k_dirs.append(k_dir)
        return args, (r, v, k_dirs, xg)

    ac, ec = prep(hc)
    al, el = prep(hl)
    yc, yl = bidirectional_scan(rwkv_scan, ac, al)

    def finish(y, r, v, k_dirs, xg):
        b, t = y.shape[:2]
        bonus = sum(jnp.sum(r * kd * r_k, axis=-1, keepdims=True) for kd in k_dirs) * v
        gate = jax.nn.sigmoid(xg @ g1) @ g2
        out = head_groupnorm(y, ln_w, ln_b) + bonus.reshape(b, t, D_MODEL)
        return (out.astype(xg.dtype) * gate) @ w_o

    return (finish(yc, *ec) if need_ctx_out else None), finish(yl, *el)


def fourier_mixer(h, out_w):
    b, t, d = h.shape
    hg = h.astype(jnp.float32).reshape(b, t, FNET_GROUPS, d // FNET_GROUPS)
    y = jnp.fft.fftn(hg, axes=(1, 3), norm='ortho').real
    return y.reshape(b, t, d).astype(h.dtype) @ out_w


def expert_choice_ffn(h, router_w, w_gate, w_up, w_down):
    b, n, d = h.shape
    cap = n * EC_CAPACITY // N_EXPERTS
    affinity = jax.nn.softmax((h @ router_w).astype(jnp.float32), axis=-1)
    gate, idx = lax.top_k(jnp.swapaxes(affinity, 1, 2), cap)
    xe = jax.vmap(lambda hb, ib: hb[ib])(h, idx)
    hid = jax.nn.silu(jnp.einsum('becd,edf->becf', xe, w_gate)) * jnp.einsum('becd,edf->becf', xe, w_up)
    ye = jnp.einsum('becf,efd->becd', hid, w_down) * gate[..., None].astype(h.dtype)
    scatter = lambda ib, yb: jnp.zeros((n, d), yb.dtype).at[ib.reshape(-1)].add(yb.reshape(-1, d))
    return jax.vmap(scatter)(idx, ye)


def ctx_needed_after(i):
    return any((j % N_MIXERS) in RECURRENT_MIXERS for j in range(i + 1, DEPTH))


def setup_inputs(seed: int = 0) -> dict:
    key = jax.random.key(seed)
    keys = iter(jax.random.split(key, 64))
    d = D_MODEL
    inv_d = d ** -0.5

    def nrm(shape, scale):
        return scale * jax.random.normal(next(keys), shape, jnp.float32)

    def unif(shape, lo, hi):
        return jax.random.uniform(next(keys), shape, jnp.float32, lo, hi)

    def gain(shape):
        return 1.0 + nrm(shape, 0.02)

    def dt_bias(shape):
        dt = jnp.exp(unif(shape, math.log(1e-3), math.log(1e-1)))
        return jnp.log(jnp.expm1(dt))

    nA, nB, nC, nD = N_LAYERS_SSM, N_LAYERS_GDN, N_LAYERS_RWKV, N_LAYERS_FNET
    return {
        'x': nrm((BATCH, SEQ, d), 1.0),
        'c': nrm((BATCH, d), 1.0),
        'ctx': nrm((BATCH, CTX_LEN, d), 1.0),
        'c_ctx': nrm((d,), 1.0),
        'ada_w': nrm((DEPTH, d, 6 * d), 0.5 * inv_d),
        'ada_b': nrm((DEPTH, 6 * d), 0.02),
        'norm1_w': gain((DEPTH, d)),
        'norm2_w': gain((DEPTH, d)),
        'router_w': nrm((DEPTH, d, N_EXPERTS), inv_d),
        'expert_w_gate': nrm((DEPTH, N_EXPERTS, d, EXPERT_FF), inv_d),
        'expert_w_up': nrm((DEPTH, N_EXPERTS, d, EXPERT_FF), inv_d),
        'expert_w_down': nrm((DEPTH, N_EXPERTS, EXPERT_FF, d), EXPERT_FF ** -0.5),
        'ssm_in_w': nrm((nA, d, SSM_PROJ), inv_d),
        'ssm_conv_w': nrm((nA, CONV_SIZE, CONV_SIZE, SSM_CONV_DIM), 1.0 / CONV_SIZE),
        'ssm_conv_b': nrm((nA, SSM_CONV_DIM), 0.02),
        'ssm_a_log': jnp.log(unif((nA, 2, SSM_HEADS), 1.0, 16.0)),
        'ssm_dt_bias': dt_bias((nA, 2, SSM_HEADS)),
        'ssm_d': gain((nA, SSM_HEADS)),
        'ssm_norm_w': gain((nA, SSM_D_INNER)),
        'ssm_out_w': nrm((nA, SSM_D_INNER, d), SSM_D_INNER ** -0.5),
        'gdn_in_w': nrm((nB, d, GDN_PROJ), inv_d),
        'gdn_conv_w': nrm((nB, CONV_SIZE, CONV_SIZE, GDN_CONV_DIM), 1.0 / CONV_SIZE),
        'gdn_a_log': jnp.log(unif((nB, 2, GDN_HEADS), 1.0, 16.0)),
        'gdn_dt_bias': dt_bias((nB, 2, GDN_HEADS)),
        'gdn_norm_w': gain((nB, GDN_DV)),
        'gdn_out_w': nrm((nB, GDN_VW, d), GDN_VW ** -0.5),
        'rwkv_mu': unif((nC, 6, d), 0.0, 1.0),
        'rwkv_w_r': nrm((nC, d, d), inv_d),
        'rwkv_w_k': nrm((nC, d, d), inv_d),
        'rwkv_w_v': nrm((nC, d, d), inv_d),
        'rwkv_w_o': nrm((nC, d, d), inv_d),
        'rwkv_w0': unif((nC, 2, d), -6.0, -1.0),
        'rwkv_w1': nrm((nC, 2, d, RWKV_LORA_W), inv_d),
        'rwkv_w2': nrm((nC, 2, RWKV_LORA_W, d), 0.1 * RWKV_LORA_W ** -0.5),
        'rwkv_a0': nrm((nC, 2, d), 0.1),
        'rwkv_a1': nrm((nC, 2, d, RWKV_LORA_A), inv_d),
        'rwkv_a2': nrm((nC, 2, RWKV_LORA_A, d), 0.5 * RWKV_LORA_A ** -0.5),
        'rwkv_g1': nrm((nC, d, RWKV_LORA_G), inv_d),
        'rwkv_g2': nrm((nC, RWKV_LORA_G, d), RWKV_LORA_G ** -0.5),
        'rwkv_k_k': 0.85 + nrm((nC, d), 0.02),
        'rwkv_k_a': gain((nC, d)),
        'rwkv_r_k': nrm((nC, RWKV_HEADS, RWKV_HEAD), 0.1),
        'rwkv_ln_w': gain((nC, d)),
        'rwkv_ln_b': nrm((nC, d), 0.02),
        'fnet_out_w': nrm((nD, d, d), inv_d),
        'final_norm_w': gain((d,)),
    }


def reference(x, c, ctx, c_ctx, ada_w, ada_b, norm1_w, norm2_w, router_w, expert_w_gate, expert_w_up, expert_w_down,
              ssm_in_w, ssm_conv_w, ssm_conv_b, ssm_a_log, ssm_dt_bias, ssm_d, ssm_norm_w, ssm_out_w,
              gdn_in_w, gdn_conv_w, gdn_a_log, gdn_dt_bias, gdn_norm_w, gdn_out_w,
              rwkv_mu, rwkv_w_r, rwkv_w_k, rwkv_w_v, rwkv_w_o, rwkv_w0, rwkv_w1, rwkv_w2, rwkv_a0, rwkv_a1, rwkv_a2,
              rwkv_g1, rwkv_g2, rwkv_k_k, rwkv_k_a, rwkv_r_k, rwkv_ln_w, rwkv_ln_b,
              fnet_out_w, final_norm_w):
    cond_lat = jax.nn.silu(c)
    cond_ctx = jax.nn.silu(c_ctx)
    h_lat, h_ctx = x, ctx
    for i in range(DEPTH):
        m, j = i % N_MIXERS, i // N_MIXERS
        ctx_out = ctx_needed_after(i)
        ctx_in = ctx_out or (m in RECURRENT_MIXERS)
        mod_l = jnp.split((cond_lat @ ada_w[i] + ada_b[i])[:, None, :], 6, axis=-1)
        hl = modulate(rmsnorm(h_lat, norm1_w[i]), mod_l[0], mod_l[1])
        hc = None
        if ctx_in:
            mod_c = jnp.split(cond_ctx @ ada_w[i] + ada_b[i], 6, axis=-1)
            hc = modulate(rmsnorm(h_ctx, norm1_w[i]), mod_c[0], mod_c[1])
        if m == 0:
            yc, yl = mamba2_mixer(hc, hl, ssm_in_w[j], ssm_conv_w[j], ssm_conv_b[j], ssm_a_log[j], ssm_dt_bias[j],
                                  ssm_d[j], ssm_norm_w[j], ssm_out_w[j], ctx_out)
        elif m == 1:
            yc, yl = gated_deltanet_mixer(hc, hl, gdn_in_w[j], gdn_conv_w[j], gdn_a_log[j], gdn_dt_bias[j],
                                          gdn_norm_w[j], gdn_out_w[j], ctx_out)
        elif m == 2:
            yc, yl = rwkv7_mixer(hc, hl, rwkv_mu[j], rwkv_w_r[j], rwkv_w_k[j], rwkv_w_v[j], rwkv_w_o[j],
                                 rwkv_w0[j], rwkv_w1[j], rwkv_w2[j], rwkv_a0[j], rwkv_a1[j], rwkv_a2[j],
                                 rwkv_g1[j], rwkv_g2[j], rwkv_k_k[j], rwkv_k_a[j], rwkv_r_k[j],
                                 rwkv_ln_w[j], rwkv_ln_b[j], ctx_out)
        else:
            yc = fourier_mixer(hc, fnet_out_w[j]) if ctx_out else None
            yl = fourier_mixer(hl, fnet_out_w[j])
        h_lat = h_lat + mod_l[2] * yl
        hl = modulate(rmsnorm(h_lat, norm2_w[i]), mod_l[3], mod_l[4])
        h_lat = h_lat + mod_l[5] * expert_choice_ffn(hl, router_w[i], expert_w_gate[i], expert_w_up[i], expert_w_down[i])
        if ctx_out:
            h_ctx = h_ctx + mod_c[2] * yc
            hc = modulate(rmsnorm(h_ctx, norm2_w[i]), mod_c[3], mod_c[4])
            h_ctx = h_ctx + mod_c[5] * expert_choice_ffn(hc, router_w[i], expert_w_gate[i], expert_w_up[i], expert_w_down[i])
    return rmsnorm(h_lat, final_norm_w)
```

```python
from contextlib import ExitStack
import math
import os
import numpy as np
import ml_dtypes
import concourse.bass as bass
import concourse.mybir as mybir
from concourse.bass_utils import run_bass_kernel_spmd

F32 = mybir.dt.float32
BF16 = mybir.dt.bfloat16
I32 = mybir.dt.int32
AF = mybir.ActivationFunctionType
ALU = mybir.AluOpType
AX = mybir.AxisListType

SEM_LIMIT = 28000
NDMA_CH = 4


class _Chan:
    def __init__(self, prog, name):
        self.prog, self.name = prog, name
        self.n = 0
        self.sem = None
        self.val = 0
        self.last = None

    def ticket(self, amt):
        if self.sem is None or self.val + amt > SEM_LIMIT:
            self.sem = self.prog.ctx.enter_context(self.prog.nc.semaphore(f"{self.name}_{self.n}"))
            self.n += 1
            self.val = 0
        self.val += amt
        self.last = (self.sem, self.val)
        return self.last


class Prog:
    ENGS = ("pe", "act", "dve", "pool", "sp")

    def __init__(self):
        self.nc = bass.Bass("TRN2", target_bir_lowering=False)
        self.ctx = ExitStack()
        self.ops = {e: [] for e in self.ENGS}
        self.chan = {e: _Chan(self, "s" + e) for e in self.ENGS}
        self.dchan = {e: [_Chan(self, f"d{e}{i}") for i in range(NDMA_CH)] for e in ("sp", "act", "pool")}
        self.drr = {e: 0 for e in ("sp", "act", "pool")}
        self.res = {}
        self.waited = {e: {} for e in self.ENGS}
        self.uid = 0
        self.out_tickets = []
        self.scopes = [self.ctx]
        self.regs = {}
        self.regs_blk = set()
        self.fence_w = self.ctx.enter_context(self.nc.sbuf_tensor("fence_w", [128, 128], BF16))
        self.fence_init = False
        self.fence_ps = {}

    def dram(self, name, shape, dt, kind="Internal"):
        return self.nc.dram_tensor(name, list(shape), dt, kind=kind)

    def sbuf(self, name, shape, dt):
        return self.scopes[-1].enter_context(self.nc.sbuf_tensor(name, list(shape), dt))

    def psum(self, name, shape, dt=F32):
        esz = 4 if dt in (F32, I32) else 2
        assert len(shape) == 2 and shape[1] * esz <= 2048
        t = self.scopes[-1].enter_context(self.nc.psum_tensor(name, [128, 2048 // esz], dt))
        return t[0:shape[0], 0:shape[1]]

    def push(self):
        self.scopes.append(ExitStack())

    def pop(self):
        self.flush(barrier=True)
        self.fence_ps.pop(len(self.scopes), None)
        self.scopes.pop().close()

    def breg(self, g, val):
        if val not in self.regs:
            self.regs[val] = g.alloc_register(f"bnd{val}")
        if val not in self.regs_blk:
            g.reg_mov(self.regs[val], val)
            self.regs_blk.add(val)
        return self.regs[val]

    def all_last(self):
        tks = [c.last for c in self.chan.values() if c.last is not None]
        for e in self.dchan:
            tks += [c.last for c in self.dchan[e] if c.last is not None]
        return tks

    @staticmethod
    def _nm(x):
        if isinstance(x, str):
            return x
        t = getattr(x, "tensor", None)
        if t is not None:
            return t.name
        return x.name

    def _need(self, eng, tk, waits):
        sem, val = tk
        k = id(sem)
        if self.waited[eng].get(k, 0) >= val:
            return
        self.waited[eng][k] = val
        waits[k] = (sem, max(val, waits.get(k, (sem, 0))[1]))

    def _deps(self, eng, reads, writes, pe_acc=False):
        waits = {}
        for r in reads:
            st = self.res.get(self._nm(r))
            if st:
                for tk in st["w"].values():
                    self._need(eng, tk, waits)
        for w in writes:
            st = self.res.get(self._nm(w))
            if st:
                for k, tk in st["w"].items():
                    if pe_acc and tk[0] is self.chan["pe"].sem:
                        continue
                    self._need(eng, tk, waits)
                for tk in st["r"].values():
                    self._need(eng, tk, waits)
        return list(waits.values())

    def _mark(self, tk, reads, writes):
        k = id(tk[0])
        for w in writes:
            self.res[self._nm(w)] = {"w": {k: tk}, "r": {}}
        for r in reads:
            st = self.res.setdefault(self._nm(r), {"w": {}, "r": {}})
            old = st["r"].get(k)
            if old is None or old[1] < tk[1]:
                st["r"][k] = tk

    def op(self, eng, fn, reads=(), writes=(), pe_acc=False):
        waits = self._deps(eng, reads, writes, pe_acc)
        tk = self.chan[eng].ticket(1)
        self._mark(tk, reads, writes)
        self.ops[eng].append((waits, fn, tk[0], 1))
        return tk

    def dma(self, out, in_, eng="sp", reads=None, writes=None, fn=None, is_output=False):
        reads = [in_] if reads is None else reads
        writes = [out] if writes is None else writes
        ch = self.dchan[eng][self.drr[eng] % NDMA_CH]
        self.drr[eng] += 1
        waits = self._deps(eng, reads, writes)
        if ch.last is not None:
            w2 = {}
            self._need(eng, ch.last, w2)
            waits += list(w2.values())
        tk = ch.ticket(16)
        self._mark(tk, reads, writes)
        if fn is None:
            fn = lambda e: e.dma_start(out=out, in_=in_, allow_slow_non_contiguous=True)
        self.ops[eng].append((waits, fn, tk[0], 16))
        if is_output:
            self.out_tickets.append(tk)
        return tk

    def _fence(self, out):
        if not self.fence_init:
            self.fence_init = True
            self.op("dve", lambda e: e.memset(self.fence_w[:], 0.0), reads=[], writes=[self.fence_w])
        if self.fence_ps.get(len(self.scopes)) is None:
            self.fence_ps[len(self.scopes)] = self.psum(f"fence_ps{self.uid}", [128, 8])
            self.uid += 1
        f, w = self.fence_ps[len(self.scopes)], self.fence_w
        return self.op("pe", lambda e: e.matmul(f[:, 0:1], w[:], w[:, 0:1], start=True, stop=True),
                       reads=[w], writes=[out, f], pe_acc=True)

    def mm(self, out, lhsT, rhs, start=True, stop=True, **kw):
        tk = self.op("pe", lambda e: e.matmul(out, lhsT, rhs, start=start, stop=stop, **kw),
                     reads=[lhsT, rhs], writes=[out], pe_acc=not start)
        if stop and lhsT.dtype == F32:
            tk = self._fence(out)
        return tk

    def mm32(self, out, lhsT, rhs, **kw):
        import os
        if os.environ.get("MM32_ONCE"):
            return self.mm(out, lhsT, rhs, **kw)
        return self.mm(out, lhsT, rhs, **kw)

    def tr(self, out, in_, ident):
        tk = self.op("pe", lambda e: e.transpose(out, in_, ident), reads=[in_, ident], writes=[out])
        if in_.dtype == F32:
            tk = self._fence(out)
        return tk

    def act(self, out, in_, func, bias=None, scale=None, accum_out=None, extra_reads=()):
        kw = {}
        rd = [in_] + list(extra_reads)
        if bias is not None:
            kw["bias"] = bias
            if not isinstance(bias, (int, float)):
                rd.append(bias)
        if scale is not None:
            kw["scale"] = scale
            if not isinstance(scale, (int, float)):
                rd.append(scale)
        wr = [out]
        if accum_out is not None:
            kw["accum_out"] = accum_out
            wr.append(accum_out)
        return self.op("act", lambda e: e.activation(out, in_, func, **kw), reads=rd, writes=wr)

    def tt(self, out, in0, in1, op, eng="dve"):
        return self.op(eng, lambda e: e.tensor_tensor(out, in0, in1, op), reads=[in0, in1], writes=[out])

    def ts(self, out, in0, s1, s2, op0, op1=None, accum_out=None, eng="dve"):
        rd = [in0] + [s for s in (s1, s2) if s is not None and not isinstance(s, (int, float))]
        wr = [out] + ([accum_out] if accum_out is not None else [])
        kw = {}
        if op1 is not None:
            kw["op1"] = op1
        if accum_out is not None:
            kw["accum_out"] = accum_out
        return self.op(eng, lambda e: e.tensor_scalar(out, in0, s1, s2, op0, **kw), reads=rd, writes=wr)

    def stt(self, out, in0, scalar, in1, op0, op1, eng="dve"):
        assert eng == "dve"
        rd = [in0, in1] + ([scalar] if not isinstance(scalar, (int, float)) else [])
        return self.op(eng, lambda e: e.scalar_tensor_tensor(out, in0, scalar, in1, op0, op1), reads=rd, writes=[out])

    def copy(self, out, in_, eng="dve"):
        if eng == "act":
            return self.act(out, in_, AF.Identity)
        return self.op(eng, lambda e: e.tensor_copy(out, in_), reads=[in_], writes=[out])

    def memset(self, out, val, eng="dve"):
        return self.op(eng, lambda e: e.memset(out, val), reads=[], writes=[out])

    def recip(self, out, in_):
        return self.op("dve", lambda e: e.reciprocal(out, in_), reads=[in_], writes=[out])

    def flush(self, barrier=True):
        nc = self.nc
        finals = {e: [] for e in self.ENGS}
        if barrier:
            tks = self.all_last()
            for e in self.ENGS:
                w = {}
                for tk in tks:
                    self._need(e, tk, w)
                finals[e] = list(w.values())
            self.res = {}
        ops = self.ops
        self.ops = {e: [] for e in self.ENGS}
        self.regs_blk = set()
        with nc.Block() as block:
            def run(engobj, name):
                for waits, fn, sem, amt in ops[name]:
                    for (s, v) in waits:
                        engobj.wait_ge(s, v)
                    fn(engobj).then_inc(sem, amt)
                for (s, v) in finals[name]:
                    engobj.wait_ge(s, v)

            @block.tensor
            def _(e):
                run(e, "pe")

            @block.scalar
            def _(e):
                run(e, "act")

            @block.vector
            def _(e):
                run(e, "dve")

            @block.gpsimd
            def _(e):
                run(e, "pool")

            @block.sync
            def _(e):
                run(e, "sp")

    def finish(self):
        self.flush(barrier=True)
        while len(self.scopes) > 1:
            self.scopes.pop().close()
        self.ctx.close()
        return self.nc


D = 1024
NCH = D // 128
EPS = 1e-6


def dview(t, pat, **kw):
    a = t.ap() if hasattr(t, "ap") and callable(getattr(t, "ap")) and not hasattr(t, "tensor") else t
    return a.rearrange(pat, **kw)


class K:
    def __init__(self, P, consts):
        self.P = P
        self.c = consts
        p = P
        self.ident32 = p.sbuf("ident32", [128, 128], F32)
        self.ident16 = p.sbuf("ident16", [128, 128], BF16)
        p.dma(self.ident32[:], consts["ident"].ap())
        p.copy(self.ident16[:], self.ident32[:])
        self.ones32 = p.sbuf("ones32", [128, 128], F32)
        p.memset(self.ones32[:], 1.0)
        self.ones16 = p.sbuf("ones16", [128, 128], BF16)
        p.memset(self.ones16[:], 1.0)
        self.n_uid = 0

    def uid(self, s):
        self.n_uid += 1
        return f"{s}{self.n_uid}"


def stage_norm(k, x_tm, T, g_bc, sh_bc, outT16=None, outT32=None, out_tm16=None, tag="n", hook=None, out_tm32=None, col0=0):
    p = k.P
    nt = T // 128
    xt = [p.sbuf(k.uid("nx"), [128, D], F32) for _ in range(2)]
    yt = [p.sbuf(k.uid("ny"), [128, D], F32) for _ in range(2)]
    y16 = [p.sbuf(k.uid("ny16"), [128, D], BF16) for _ in range(2)]
    ss = [p.sbuf(k.uid("nss"), [128, 1], F32) for _ in range(2)]
    rs = [p.sbuf(k.uid("nrs"), [128, 1], F32) for _ in range(2)]
    ps = [p.psum(k.uid("nps"), [128, 512], F32) for _ in range(2)]
    hT16 = [p.sbuf(k.uid("nh16"), [128, NCH, 128], BF16) for _ in range(2)]
    hT32 = [p.sbuf(k.uid("nh32"), [128, NCH, 128], F32) for _ in range(2)]
    xv = x_tm.ap().rearrange("(n p) d -> n p d", p=128)
    for i in range(nt):
        b = i % 2
        p.dma(xt[b][:], xv[i])
        p.act(yt[b][:], xt[b][:], AF.Square, accum_out=ss[b][:])
        p.ts(rs[b][:], ss[b][:], 1.0 / D, EPS, ALU.mult, ALU.add)
        p.act(rs[b][:], rs[b][:], AF.Sqrt)
        p.recip(rs[b][:], rs[b][:])
        p.stt(yt[b][:], xt[b][:], rs[b][:], g_bc[:], ALU.mult, ALU.mult)
        p.tt(yt[b][:], yt[b][:], sh_bc[:], ALU.add, eng="pool")
        if out_tm32 is not None:
            p.dma(out_tm32.ap().rearrange("(n p) d -> n p d", p=128)[i], yt[b][:], eng="act")
        if out_tm16 is not None:
            p.copy(y16[b][:], yt[b][:], eng="act")
            p.dma(out_tm16.ap().rearrange("(n p) d -> n p d", p=128)[i], y16[b][:], eng="act")
        if outT16 is not None or outT32 is not None or hook is not None:
            for h in range(2):
                for c in range(4):
                    cc = h * 4 + c
                    p.tr(ps[h][:, c * 128:(c + 1) * 128], yt[b][:, cc * 128:(cc + 1) * 128], k.ident32[:])
                if outT16 is not None:
                    p.copy(hT16[b][:, h * 4:(h + 1) * 4, :], ps[h][:].rearrange("p (c t) -> p c t", c=4), eng="dve")
                if outT32 is not None or hook is not None:
                    p.copy(hT32[b][:, h * 4:(h + 1) * 4, :], ps[h][:].rearrange("p (c t) -> p c t", c=4), eng="dve")
            if outT16 is not None:
                p.dma(outT16.ap().rearrange("(c p) t -> p c t", p=128)[:, :, col0 + i * 128:col0 + (i + 1) * 128], hT16[b][:])
            if outT32 is not None:
                p.dma(outT32.ap().rearrange("(c p) t -> p c t", p=128)[:, :, i * 128:(i + 1) * 128], hT32[b][:])
            if hook is not None:
                hook(i, yt[b], hT32[b])


def gemm(k, W, xT, T, Kd, N, epi, ng=8, tb=512, wdt=BF16, tag="g"):
    p = k.P
    kc = Kd // 128
    nn = (N + 127) // 128
    wt = [p.sbuf(k.uid("gw"), [128, kc, ng * 128], wdt) for _ in range(2)]
    xb = [p.sbuf(k.uid("gx"), [128, kc, tb], wdt) for _ in range(2)]
    ps = [p.psum(k.uid("gps"), [128, tb], F32) for _ in range(2)]
    xv = xT.ap().rearrange("(c p) t -> p c t", p=128)
    Wv = W.rearrange("(c p) n -> p c n", p=128)
    gi = 0
    cnt = 0
    for n0 in range(0, nn, ng):
        n1 = min(nn, n0 + ng)
        c0, c1 = n0 * 128, min(N, n1 * 128)
        wb = wt[gi % 2]
        gi += 1
        if wdt == F32:
            p.dma(wb[:, :, 0:c1 - c0], Wv[:, :, c0:c1], eng="sp")
        else:
            p.dma(wb[:, :, 0:c1 - c0], Wv[:, :, c0:c1], eng="pool")
        for t0 in range(0, T, tb):
            ntok = min(tb, T - t0)
            xx = xb[cnt % 2]
            p.dma(xx[:, :, 0:ntok], xv[:, :, t0:t0 + ntok], eng="sp")
            for n in range(n0, n1):
                nsz = min(128, N - n * 128)
                pp = ps[cnt % 2]
                cnt += 1
                for kk in range(kc):
                    p.mm(pp[0:nsz, 0:ntok], wb[:, kk, (n - n0) * 128:(n - n0) * 128 + nsz], xx[:, kk, 0:ntok],
                         start=(kk == 0), stop=(kk == kc - 1))
                epi(pp[0:nsz, 0:ntok], n, nsz, t0, ntok)


def epi_store(k, outT, dt, act_fn=None, eng="dve", row0=0):
    p = k.P
    ob = [p.sbuf(k.uid("eo"), [128, 512], dt) for _ in range(3)]
    st = {"i": 0}

    def epi(ps, n, nsz, t0, ntok):
        o = ob[st["i"] % 3]
        st["i"] += 1
        if act_fn is not None:
            p.act(o[0:nsz, 0:ntok], ps, act_fn)
        else:
            p.copy(o[0:nsz, 0:ntok], ps, eng=eng)
        p.dma(outT.ap()[row0 + n * 128:row0 + n * 128 + nsz, t0:t0 + ntok], o[0:nsz, 0:ntok], eng="act")
    return epi


NE = 16
FF = 1024
BIG = 1.0e6


def _dbg(i, e_, b, f):
    try:
        return f()
    except Exception:
        print("DBG fail at", i, e_, b)
        raise


def bc_row(k, name, row_ap, n=D, parts=128, eng="sp"):
    t = k.P.sbuf(k.uid(name), [parts, n], F32)
    k.P.dma(t[:], row_ap.partition_broadcast(parts), eng=eng)
    return t


def mod_tiles(k, nw_row, modT, r, j_shift, j_scale):
    p = k.P
    nw = bc_row(k, "nw", nw_row)
    sc = bc_row(k, "sc", modT.ap()[r:r + 1, j_scale * D:(j_scale + 1) * D])
    sh = bc_row(k, "sh", modT.ap()[r:r + 1, j_shift * D:(j_shift + 1) * D])
    p.stt(sc[:], sc[:], 1.0, nw[:], ALU.add, ALU.mult)
    return sc, sh


def stage_ada(k, condT, ada_w, ada_b, modT):
    p = k.P
    p.push()
    cs = p.sbuf(k.uid("cond"), [128, NCH, 2], F32)
    p.dma(cs[:], condT.ap().rearrange("(c p) j -> p c j", p=128))
    p.act(cs[:], cs[:], AF.Silu)
    bb = p.sbuf(k.uid("adab"), [2, 6 * D], F32)
    p.dma(bb[:], ada_b.partition_broadcast(2))
    wt = [p.sbuf(k.uid("adaw"), [128, NCH, 512], F32) for _ in range(2)]
    ps = [p.psum(k.uid("adaps"), [128, 512], F32) for _ in range(2)]
    ot = p.sbuf(k.uid("adao"), [2, 6 * D], F32)
    wv = ada_w.rearrange("(c p) n -> p c n", p=128)
    for j in range(12):
        w = wt[j % 2]
        p.dma(w[:], wv[:, :, j * 512:(j + 1) * 512], eng=("sp" if j % 2 == 0 else "act"))
        for c in range(NCH):
            p.mm(ps[j % 2][0:2, :], cs[:, c, :], w[:, c, :], start=(c == 0), stop=(c == NCH - 1))
        p.tt(ot[:, j * 512:(j + 1) * 512], ps[j % 2][0:2, :], bb[:, j * 512:(j + 1) * 512], ALU.add)
    p.dma(modT.ap(), ot[:])
    p.pop()


def stage_moe(k, h_tm, T, cap, nw_row, modT, r, router_w, wg, wu, wd, tokid):
    p = k.P
    nt = T // 128
    h2 = p.dram(k.uid("moe_h2"), [T, D], BF16)
    slots = [p.dram(k.uid("moe_slots"), [cap, 2], F32) for _ in range(NE)]
    p.push()
    g_bc, sh_bc = mod_tiles(k, nw_row, modT, r, 3, 4)
    wr = p.sbuf(k.uid("wr"), [128, NCH, NE], F32)
    p.dma(wr[:], router_w.rearrange("(c p) e -> p c e", p=128))
    affall = p.sbuf(k.uid("affall"), [128, nt, NE], F32)
    affT = p.sbuf(k.uid("affT"), [NE, T], F32)
    psl = p.psum(k.uid("psl"), [128, NE], F32)
    pst = p.psum(k.uid("pst"), [NE, 128], F32)
    mx = p.sbuf(k.uid("mx"), [128, 1], F32)
    sm = p.sbuf(k.uid("sm"), [128, 1], F32)
    ex = p.sbuf(k.uid("ex"), [128, NE], F32)

    def hook(i, yt, hT32):
        for c in range(NCH):
            p.mm(psl[:], hT32[:, c, :], wr[:, c, :], start=(c == 0), stop=(c == NCH - 1))
        p.op("dve", lambda e: e.reduce_max(mx[:], psl[:], AX.X), reads=[psl], writes=[mx])
        p.ts(mx[:], mx[:], -1.0, None, ALU.mult)
        p.act(ex[:], psl[:], AF.Exp, bias=mx[:], accum_out=sm[:])
        p.recip(sm[:], sm[:])
        p.ts(affall[:, i, :], ex[:], sm[:], None, ALU.mult)
        p.tr(pst[:], affall[:, i, :], k.ident32[:])
        p.copy(affT[:, i * 128:(i + 1) * 128], pst[:])

    stage_norm(k, h_tm, T, g_bc, sh_bc, out_tm16=h2, hook=hook)
    lo = p.sbuf(k.uid("lo"), [NE, 1], F32)
    hi = p.sbuf(k.uid("hi"), [NE, 1], F32)
    mid = p.sbuf(k.uid("mid"), [NE, 1], F32)
    cnt = p.sbuf(k.uid("cnt"), [NE, 1], F32)
    ge = p.sbuf(k.uid("ge"), [NE, 1], F32)
    dd = p.sbuf(k.uid("dd"), [NE, 1], F32)
    p.push()
    junk = p.sbuf(k.uid("bjunk"), [NE, T], BF16)
    p.memset(lo[:], 0.0)
    p.memset(hi[:], 1.0)
    for it in range(32):
        p.stt(mid[:], lo[:], 1.0, hi[:], ALU.mult, ALU.add)
        p.ts(mid[:], mid[:], 0.5, None, ALU.mult)
        p.ts(junk[:], affT[:], mid[:], 0.0, ALU.is_ge, ALU.add, accum_out=cnt[:])
        p.ts(ge[:], cnt[:], float(cap), None, ALU.is_ge)
        p.tt(dd[:], mid[:], lo[:], ALU.subtract)
        p.stt(lo[:], dd[:], ge[:], lo[:], ALU.mult, ALU.add)
        p.tt(dd[:], mid[:], hi[:], ALU.subtract)
        p.ts(ge[:], ge[:], -1.0, 1.0, ALU.mult, ALU.add)
        p.stt(hi[:], dd[:], ge[:], hi[:], ALU.mult, ALU.add)
    p.pop()
    SEG = min(T, 2048)
    ones = p.sbuf(k.uid("ones"), [NE, SEG], BF16)
    p.memset(ones[:], 1.0)
    mask = p.sbuf(k.uid("mask"), [NE, SEG], BF16)
    cum = [p.sbuf(k.uid("cum"), [NE, SEG], F32) for _ in range(2)]
    dest = p.sbuf(k.uid("dest"), [NE, SEG], F32)
    psd = p.psum(k.uid("psd"), [128, NE], F32)
    desti = [p.sbuf(k.uid("desti"), [128, NE], I32) for _ in range(2)]
    src = [p.sbuf(k.uid("src"), [128, NE, 2], F32) for _ in range(2)]
    tid = p.sbuf(k.uid("tid"), [128, nt], F32)
    p.dma(tid[:], tokid.ap()[:, 0:nt])
    for sgi in range(T // SEG):
        c_ = cum[sgi % 2]
        p.ts(mask[:], affT[:, sgi * SEG:(sgi + 1) * SEG], lo[:], None, ALU.is_ge)
        init = 0.0 if sgi == 0 else cum[(sgi - 1) % 2][:, SEG - 1:SEG]
        rd = [ones, mask] + ([] if sgi == 0 else [cum[(sgi - 1) % 2]])
        p.op("dve", lambda e, c_=c_, init=init: e.tensor_tensor_scan(c_[:], ones[:], mask[:], init, ALU.mult, ALU.add),
             reads=rd, writes=[c_])
        p.stt(dest[:], mask[:], -BIG, c_[:], ALU.mult, ALU.add)
        p.ts(dest[:], dest[:], BIG - 1.0, None, ALU.add)
        for ti in range(SEG // 128):
            i = sgi * (SEG // 128) + ti
            b = i % 2
            p.tr(psd[:], dest[:, ti * 128:(ti + 1) * 128], k.ident32[0:NE, 0:NE])
            p.copy(desti[b][:], psd[:])
            p.copy(src[b][:, :, 1:2], affall[:, i, :].unsqueeze(2), eng="pool")
            p.ts(src[b][:, :, 0:1], src[b][:, :, 1:2], 0.0, tid[:, i:i + 1], ALU.mult, ALU.add)
            for e_ in range(NE):
                p.dma(slots[e_].ap(), src[b][:, e_, :], eng="pool", reads=[src[b], desti[b]], writes=[slots[e_]],
                      fn=lambda g, e_=e_, b=b, i=i: _dbg(i, e_, b, lambda: g.indirect_dma_start(
                          out=slots[e_].ap(), out_offset=bass.IndirectOffsetOnAxis(ap=desti[b][:, e_:e_ + 1], axis=0),
                          in_=src[b][:, e_, :], in_offset=None, bounds_check=p.breg(g, cap - 1), oob_is_err=False)))
    p.pop()
    p.push()
    m5 = bc_row(k, "m5", modT.ap()[r:r + 1, 5 * D:6 * D])
    wsb = [p.sbuf(k.uid("wexp"), [128, NCH, FF], BF16) for _ in range(3)]
    sl = [p.sbuf(k.uid("sl"), [128, 2], F32) for _ in range(2)]
    idxi = [p.sbuf(k.uid("idxi"), [128, 1], I32) for _ in range(2)]
    gcol = [p.sbuf(k.uid("gcol"), [128, 1], F32) for _ in range(8)]
    icol = [p.sbuf(k.uid("icol"), [128, 1], I32) for _ in range(8)]
    xe = [p.sbuf(k.uid("xe"), [128, D], BF16) for _ in range(2)]
    xeT = p.sbuf(k.uid("xeT"), [128, NCH, 512], BF16)
    hid = p.sbuf(k.uid("hid"), [128, NCH, 512], BF16)
    sg = [p.sbuf(k.uid("sg"), [128, 512], F32) for _ in range(2)]
    yo = [p.sbuf(k.uid("yo"), [128, D], F32) for _ in range(2)]
    pstr = p.psum(k.uid("pstr"), [128, D], BF16)
    psg = [p.psum(k.uid("psg"), [128, 512], F32) for _ in range(2)]
    psu = [p.psum(k.uid("psu"), [128, 512], F32) for _ in range(2)]
    pso = [p.psum(k.uid("pso"), [128, 512], F32) for _ in range(2)]
    blk = min(cap, 512)
    nblk = cap // blk
    ntile = (blk + 127) // 128
    q = 0
    for e_ in range(NE):
        for wi, wsrc in enumerate((wg, wu, wd)):
            v = wsrc[e_].rearrange("(c p) f -> p c f", p=128)
            for hh in range(2):
                p.dma(wsb[wi][:, hh * 4:(hh + 1) * 4, :], v[:, hh * 4:(hh + 1) * 4, :], eng="pool")
        for bi in range(nblk):
            cols = []
            for tj in range(ntile):
                s0 = bi * blk + tj * 128
                ns = min(128, cap - s0)
                b = q % 2
                q += 1
                gc, ic = gcol[q % 8], icol[q % 8]
                p.dma(sl[b][0:ns, :], slots[e_].ap()[s0:s0 + ns, :])
                p.copy(ic[0:ns, :], sl[b][0:ns, 0:1])
                p.copy(gc[0:ns, :], sl[b][0:ns, 1:2], eng="pool")
                p.dma(xe[b][0:ns, :], h2.ap(), eng="pool", reads=[h2, ic], writes=[xe[b]],
                      fn=lambda g, b=b, ic=ic, ns=ns: g.indirect_dma_start(
                          out=xe[b][0:ns, :], out_offset=None, in_=h2.ap(),
                          in_offset=bass.IndirectOffsetOnAxis(ap=ic[0:ns, 0:1], axis=0),
                          bounds_check=p.breg(g, T - 1), oob_is_err=False))
                for c in range(NCH):
                    p.tr(pstr[:, c * 128:c * 128 + ns], xe[b][0:ns, c * 128:(c + 1) * 128], k.ident16[0:ns, 0:ns])
                p.copy(xeT[:, :, tj * 128:tj * 128 + ns], pstr[:].rearrange("p (c t) -> p c t", c=NCH)[:, :, 0:ns])
                cols.append((tj, ns, gc, ic))
            nb = sum(c[1] for c in cols)
            for f in range(NCH):
                pg, pu = psg[f % 2], psu[f % 2]
                for c in range(NCH):
                    p.mm(pg[:, 0:nb], wsb[0][:, c, f * 128:(f + 1) * 128], xeT[:, c, 0:nb], start=(c == 0), stop=(c == NCH - 1))
                for c in range(NCH):
                    p.mm(pu[:, 0:nb], wsb[1][:, c, f * 128:(f + 1) * 128], xeT[:, c, 0:nb], start=(c == 0), stop=(c == NCH - 1))
                p.act(sg[f % 2][:, 0:nb], pg[:, 0:nb], AF.Silu)
                p.tt(hid[:, f, 0:nb], sg[f % 2][:, 0:nb], pu[:, 0:nb], ALU.mult)
            for (tj, ns, gc, ic) in cols:
                y = yo[tj % 2]
                for hh in range(2):
                    po = pso[hh]
                    for f in range(NCH):
                        p.mm(po[0:ns, :], hid[:, f, tj * 128:tj * 128 + ns], wsb[2][:, f, hh * 512:(hh + 1) * 512],
                             start=(f == 0), stop=(f == NCH - 1))
                    p.stt(y[0:ns, hh * 512:(hh + 1) * 512], po[0:ns, :], gc[0:ns, :], m5[0:ns, hh * 512:(hh + 1) * 512],
                          ALU.mult, ALU.mult)
                p.dma(h_tm.ap(), y[0:ns, :], eng="pool", reads=[y, ic], writes=[h_tm],
                      fn=lambda g, y=y, ic=ic, ns=ns: g.indirect_dma_start(
                          out=h_tm.ap(), out_offset=bass.IndirectOffsetOnAxis(ap=ic[0:ns, 0:1], axis=0),
                          in_=y[0:ns, :], in_offset=None, bounds_check=p.breg(g, T - 1), oob_is_err=False, compute_op=ALU.add))
    p.pop()


NEG = -1.0e30


def host_consts():
    i = np.arange(128)
    tri_f = (i[:, None] <= i[None, :]).astype(np.float32)
    tri_b = (i[:, None] >= i[None, :]).astype(np.float32)
    c = {
        "ident": np.eye(128, dtype=np.float32),
        "tokid": (np.arange(128)[None, :] * 128 + np.arange(128)[:, None]).astype(np.float32),
        "tri_f": tri_f, "tri_b": tri_b,
        "nm_f": np.where(tri_f > 0, 0.0, NEG).astype(np.float32),
        "nm_b": np.where(tri_b > 0, 0.0, NEG).astype(np.float32),
        "sel_f": np.zeros((128, 128), np.float32), "sel_b": np.zeros((128, 128), np.float32),
        "selh": (np.arange(128)[:, None, None] == np.arange(32)[None, :, None]).astype(np.float32) * np.ones((1, 1, 128), np.float32),
    }
    c["sel_f"][127, :] = 1.0
    c["sel_b"][0, :] = 1.0
    return c


def load_const(k, name, shape=None, dt=F32):
    h = k.c[name]
    t = k.P.sbuf(k.uid("c_" + name), list(h.shape) if shape is None else shape, dt)
    if dt == F32:
        k.P.dma(t[:], h.ap())
    else:
        k.P.dma(t[:], h.ap(), eng="pool")
    return t


def gemm_tm(k, W, xT, t_lo, t_hi, Kd, N, epi):
    p = k.P
    kc = Kd // 128
    wt = p.sbuf(k.uid("tw"), [128, kc, N], BF16)
    Wv = W.rearrange("(c p) n -> p c n", p=128)
    for c0 in range(0, kc, 4):
        p.dma(wt[:, c0:c0 + 4, :], Wv[:, c0:c0 + 4, :], eng="pool")
    xb = [p.sbuf(k.uid("tx"), [128, kc, 128], BF16) for _ in range(2)]
    ps = [p.psum(k.uid("tps"), [128, 512], F32) for _ in range(2)]
    xv = xT.ap().rearrange("(c p) t -> p c t", p=128)
    q = 0
    for i, t0 in enumerate(range(t_lo, t_hi, 128)):
        xx = xb[i % 2]
        p.dma(xx[:], xv[:, :, t0:t0 + 128])
        for c0 in range(0, N, 512):
            pp = ps[q % 2]
            q += 1
            for kk in range(kc):
                p.mm(pp[:], xx[:, kk, :], wt[:, kk, c0:c0 + 512], start=(kk == 0), stop=(kk == kc - 1))
            epi(pp, t0, c0)


def stage_outproj(k, yT, Kd, W, h_tm, modT, r, t_lo, t_hi):
    p = k.P
    p.push()
    m2 = bc_row(k, "m2", modT.ap()[r:r + 1, 2 * D:3 * D])
    ht = [p.sbuf(k.uid("oh"), [128, D], F32) for _ in range(2)]
    hv = h_tm.ap().rearrange("(n p) d -> n p d", p=128)
    tmp2 = [p.sbuf(k.uid("ot"), [128, 512], F32) for _ in range(2)]
    cnt = {"q": 0}

    def epi2(ps, t0, c0):
        i = (t0 - t_lo) // 128
        b = i % 2
        if c0 == 0:
            p.dma(ht[b][:], hv[i], eng="act")
        tq = tmp2[cnt["q"] % 2]
        cnt["q"] += 1
        p.tt(tq[:], ps[:], m2[:, c0:c0 + 512], ALU.mult)
        p.tt(ht[b][:, c0:c0 + 512], ht[b][:, c0:c0 + 512], tq[:], ALU.add, eng="pool")
        if c0 + 512 >= D:
            p.dma(hv[i], ht[b][:], eng="act")

    gemm_tm(k, W, yT, t_lo, t_hi, Kd, D, epi2)
    p.pop()


def conv_stage(k, xin, xout, c_lo, c_hi, w9, bias, Tc, T, act=AF.Silu, rows_blk=64):
    p = k.P
    p.push()
    G = 64
    rows = T // G
    rb = min(rows, rows_blk)
    xi = [p.sbuf(k.uid("cvi"), [128, (rb + 2) * G], xin.dtype) for _ in range(2)]
    acc = [p.sbuf(k.uid("cva"), [128, rb * G], F32) for _ in range(2)]
    xo = [p.sbuf(k.uid("cvo"), [128, rb * G], BF16) for _ in range(2)]
    ci = p.sbuf(k.uid("cci"), [128, Tc + 2], xin.dtype)
    ca = p.sbuf(k.uid("cca"), [128, Tc], F32)
    co = p.sbuf(k.uid("cco"), [128, Tc], BF16)
    nch = (c_hi - c_lo) // 128
    wt = p.sbuf(k.uid("cvw"), [128, nch, 9], F32)
    p.dma(wt[:], w9[c_lo:c_hi, :].rearrange("(c p) j -> p c j", p=128))
    bt = p.sbuf(k.uid("cvb"), [128, nch, 1], F32)
    if bias is not None:
        p.dma(bt[:], bias[c_lo:c_hi, :].rearrange("(c p) j -> p c j", p=128))
    else:
        p.memset(bt[:], 0.0)
    q = 0
    for c in range(nch):
        r0c = c_lo + c * 128
        if Tc > 0:
            p.memset(ci[:, 0:1], 0.0)
            p.memset(ci[:, Tc + 1:Tc + 2], 0.0)
            p.dma(ci[:, 1:Tc + 1], xin.ap()[r0c:r0c + 128, 0:Tc])
            p.ts(ca[:], ci[:, 1:Tc + 1], wt[:, c, 4:5], None, ALU.mult)
            p.stt(ca[:], ci[:, 0:Tc], wt[:, c, 3:4], ca[:], ALU.mult, ALU.add)
            p.stt(ca[:], ci[:, 2:Tc + 2], wt[:, c, 5:6], ca[:], ALU.mult, ALU.add)
            p.act(co[:], ca[:], act, bias=bt[:, c, :])
            p.dma(xout.ap()[r0c:r0c + 128, 0:Tc], co[:], eng="act")
        for r0 in range(0, rows, rb):
            b = q % 2
            eng = "dve"
            q += 1
            X = xi[b][:].rearrange("p (r g) -> p r g", g=G)
            A = acc[b][:].rearrange("p (r g) -> p r g", g=G)
            lo_r = max(r0 - 1, 0)
            hi_r = min(r0 + rb + 1, rows)
            if r0 == 0:
                p.memset(xi[b][:, 0:G], 0.0, eng=eng)
            if r0 + rb >= rows:
                p.memset(xi[b][:, (rb + 1) * G:(rb + 2) * G], 0.0, eng=eng)
            p.dma(xi[b][:, (lo_r - r0 + 1) * G:(hi_r - r0 + 1) * G], xin.ap()[r0c:r0c + 128, Tc + lo_r * G:Tc + hi_r * G])
            p.ts(A[:, :, :], X[:, 1:rb + 1, :], wt[:, c, 4:5], None, ALU.mult, eng=eng)
            for dy in range(3):
                for dx in range(3):
                    if dy == 1 and dx == 1:
                        continue
                    c0 = 1 if dx == 0 else 0
                    c1 = G - 1 if dx == 2 else G
                    p.stt(A[:, :, c0:c1], X[:, dy:dy + rb, c0 + dx - 1:c1 + dx - 1], wt[:, c, dy * 3 + dx:dy * 3 + dx + 1],
                          A[:, :, c0:c1], ALU.mult, ALU.add, eng=eng)
            p.act(xo[b][:], acc[b][:], act, bias=bt[:, c, :])
            p.dma(xout.ap()[r0c:r0c + 128, Tc + r0 * G:Tc + (r0 + rb) * G], xo[b][:], eng="act")
    p.pop()


def chunk_order(Tc, T, d):
    cc = list(range(0, Tc, 128))
    lc = list(range(Tc, Tc + T, 128))
    return (cc + lc) if d == 0 else (cc[::-1] + lc[::-1])


def decay_prep(k, d, H, rawT, row0, t0, bias_bc, negA_bc, C, bufs, softplus=True):
    p = k.P
    tri, sel = (C["tri_f"], C["sel_f"]) if d == 0 else (C["tri_b"], C["sel_b"])
    raw = bufs["raw"]
    p.dma(raw[0:H, :], rawT.ap()[row0:row0 + H, t0:t0 + 128])
    ps = bufs["ps_small"]
    ps2 = bufs["ps_small2"]
    p.tr(ps2[:], raw[:], k.ident32[:])
    dt = bufs["dt_tm"]
    p.tt(dt[:, 0:H], ps2[:, 0:H], bias_bc[:, 0:H], ALU.add)
    p.act(dt[:, 0:H], dt[:, 0:H], AF.Exp)
    p.act(dt[:, 0:H], dt[:, 0:H], AF.Ln, bias=1.0)
    la = bufs["la_tm"]
    p.tt(la[:, 0:H], dt[:, 0:H], negA_bc[:, 0:H], ALU.mult)
    p.mm32(ps[:], tri[:], la[:])
    ncs = bufs["ncs_tm"]
    cs = bufs["cs_tm"]
    p.copy(cs[:, 0:H], ps[:, 0:H], eng="act")
    p.act(ncs[:, 0:H], cs[:, 0:H], AF.Identity, scale=-1.0)
    p.mm32(ps2[:], la[:], tri[:])
    csT = bufs["csT"]
    p.copy(csT[:], ps2[:])
    p.mm32(ps[:], sel[:], cs[:])
    dl = bufs["dl_bc"]
    p.act(dl[:, 0:H], ps[:, 0:H], AF.Exp)
    wx = bufs["wx_tm"]
    p.tt(wx[:, 0:H], ps[:, 0:H], cs[:, 0:H], ALU.subtract)
    p.act(wx[:, 0:H], wx[:, 0:H], AF.Exp)
    return dt, ncs, csT, wx, dl


def decay_bufs(k, H=32):
    p = k.P
    b = {n: p.sbuf(k.uid(n), [128, 128], F32) for n in ("dt_tm", "la_tm", "ncs_tm", "cs_tm", "dl_bc", "wx_tm", "raw", "csT")}
    for n in ("la_tm", "cs_tm", "raw"):
        p.memset(b[n][:], 0.0)
    b["ps_small"] = p.psum(k.uid("pss"), [128, 128], F32)
    b["ps_small2"] = p.psum(k.uid("pss2"), [128, 128], F32)
    return b


def stage_ssd(k, hT, Tc, T, W, modT, h_lat, h_ctx, ctx_out):
    p = k.P
    Tt = Tc + T
    DI, HD, H, G, NS = 2048, 64, 32, 8, 128
    szT = p.dram(k.uid("ssd_sz"), [DI, Tt], BF16)
    xbcT = p.dram(k.uid("ssd_xbc"), [4096, Tt], BF16)
    xcT = p.dram(k.uid("ssd_xc"), [4096, Tt], BF16)
    dtT = p.dram(k.uid("ssd_dt"), [64, Tt], F32)
    yfT = p.dram(k.uid("ssd_yf"), [DI, Tt], F32)
    ygT = p.dram(k.uid("ssd_yg"), [DI, Tt], F32)
    ynT = p.dram(k.uid("ssd_yn"), [DI, Tt], BF16)
    k.dbg = dict(szT=szT, xbcT=xbcT, xcT=xcT, dtT=dtT, yfT=yfT, ygT=ygT, ynT=ynT)
    p.push()
    e_z = epi_store(k, szT, BF16, act_fn=AF.Silu)
    e_x = epi_store(k, xbcT, BF16, row0=-16 * 128)
    e_d = epi_store(k, dtT, F32, row0=-48 * 128)

    def epi(ps, n, nsz, t0, ntok):
        (e_z if n < 16 else (e_x if n < 48 else e_d))(ps, n, nsz, t0, ntok)
    gemm(k, W["in_w"], hT, Tt, D, 6208, epi)
    p.pop()
    import os
    STOP = int(os.environ.get("SSD_STOP", "9"))
    if STOP <= 1:
        return
    conv_stage(k, xbcT, xcT, 0, 4096, W["conv_w"], W["conv_b"], Tc, T)
    if STOP <= 2:
        return
    p.push()
    C = {n: load_const(k, n) for n in ("tri_f", "tri_b", "nm_f", "nm_b", "sel_f", "sel_b")}
    selh = load_const(k, "selh")
    bufs = decay_bufs(k)
    ST32 = p.sbuf(k.uid("ST32"), [128, G, 256], F32)
    STp = p.sbuf(k.uid("STp"), [128, G * 4, 128], BF16)
    xpad = [p.sbuf(k.uid("xpad"), [128, 4, 128], BF16) for _ in range(2)]
    for t_ in xpad:
        p.memset(t_[:], 0.0)
    xTt = [p.sbuf(k.uid("xTt"), [128, 2, 128], BF16) for _ in range(2)]
    BCt = [p.sbuf(k.uid("BCt"), [128, 2, 128], BF16) for _ in range(2)]
    Btm = [p.sbuf(k.uid("Btm"), [128, 128], BF16) for _ in range(2)]
    xtm = [p.sbuf(k.uid("xtm"), [128, 256], BF16) for _ in range(2)]
    cbm = [p.sbuf(k.uid("cbm"), [128, 128], F32) for _ in range(2)]
    cbd = [p.sbuf(k.uid("cbd"), [128, 128], F32) for _ in range(2)]
    xw = [p.sbuf(k.uid("xw"), [128, 256], BF16) for _ in range(2)]
    dec = [p.sbuf(k.uid("dec"), [128, 128], F32) for _ in range(2)]
    ebc = [p.sbuf(k.uid("ebc"), [128, 128], F32) for _ in range(2)]
    MT = [p.sbuf(k.uid("MT"), [128, 128], BF16) for _ in range(4)]
    CTs = [p.sbuf(k.uid("CTs"), [128, 128], BF16) for _ in range(4)]
    yo = [p.sbuf(k.uid("yo"), [128, 128], F32) for _ in range(2)]
    yf = [p.sbuf(k.uid("yf"), [128, 128], F32) for _ in range(2)]
    zt = [p.sbuf(k.uid("zt"), [128, 128], BF16) for _ in range(2)]
    ps_tr = p.psum(k.uid("ps_tr"), [128, 512], BF16)
    ps_a = [p.psum(k.uid("ps_a"), [128, 256], F32) for _ in range(2)]
    ps_y = p.psum(k.uid("ps_y"), [128, 256], F32)
    ps_s_ = p.psum(k.uid("ps_s"), [128, 512], F32)
    ps_s, ps_cb = ps_s_[:, 0:256], ps_s_[:, 256:384]
    wxd = p.sbuf(k.uid("wxd"), [128, H], F32)
    ncs_snap = p.sbuf(k.uid("ncs_snap"), [128, 32], F32)
    dtb = p.sbuf(k.uid("dtb"), [128, 64], F32)
    p.dma(dtb[:], W["dt_bias"].partition_broadcast(128))
    nA = p.sbuf(k.uid("nA"), [128, 64], F32)
    p.dma(nA[:], W["a_log"].partition_broadcast(128))
    p.act(nA[:], nA[:], AF.Exp)
    p.ts(nA[:], nA[:], -1.0, None, ALU.mult)
    dsk = p.sbuf(k.uid("dsk"), [128, 16, 1], F32)
    p.dma(dsk[:], W["d_rep"].rearrange("(c p) o -> p c o", p=128))
    q = 0
    for d in range(2):
        tri, nm = (C["tri_f"], C["nm_f"]) if d == 0 else (C["tri_b"], C["nm_b"])
        p.memset(ST32[:], 0.0)
        p.memset(STp[:], 0.0)
        p.ts(ST32[:], ST32[:], 1.0, None, ALU.mult)
        p.ts(STp[:], STp[:], 1.0, None, ALU.mult)
        for t0 in chunk_order(Tc, T, d)[:int(os.environ.get("SSD_NCH", "9999"))]:
            dt, ncs, csT, wx, dl = decay_prep(k, d, H, dtT, d * H, t0, dtb[:, d * H:(d + 1) * H], nA[:, d * H:(d + 1) * H], C, bufs)
            p.tt(wxd[:], wx[:, 0:H], dt[:, 0:H], ALU.mult)
            if os.environ.get("SSD_DBG2"):
                p.copy(ncs_snap[:, 0:H], ncs[:, 0:H])
            LV = int(os.environ.get("SSD_LV", "9"))
            for g in range(int(os.environ.get("SSD_G", G)) if LV >= 2 else 0):
                b = q % 2
                q += 1
                p.dma(xTt[b][:], xcT.ap()[g * 256:(g + 1) * 256, t0:t0 + 128].rearrange("(c p) t -> p c t", p=128))
                p.dma(BCt[b][:, 0, :], xcT.ap()[2048 + g * 128:2048 + (g + 1) * 128, t0:t0 + 128])
                p.dma(BCt[b][:, 1, :], xcT.ap()[3072 + g * 128:3072 + (g + 1) * 128, t0:t0 + 128])
                SUB = int(os.environ.get("SSD_SUB", "9"))
                if SUB >= 2:
                    for c in range(2):
                        p.tr(ps_tr[:, c * 128:(c + 1) * 128], xTt[b][:, c, :], k.ident16[:])
                    p.tr(ps_tr[:, 256:384], BCt[b][:, 0, :], k.ident16[:])
                if SUB >= 3:
                    p.copy(xtm[b][:], ps_tr[:, 0:256])
                    for j in range(4):
                        p.copy(xpad[b][:, j, (j % 2) * 64:(j % 2) * 64 + 64], xtm[b][:, j * 64:(j + 1) * 64], eng=("act" if j % 2 else "dve"))
                    p.copy(Btm[b][:], ps_tr[:, 256:384])
                if SUB >= 4:
                    for j in range(4):
                        h = g * 4 + j
                        p.ts(xw[b][:, j * 64:(j + 1) * 64], xtm[b][:, j * 64:(j + 1) * 64], wxd[:, h:h + 1], None, ALU.mult)
                if SUB >= 5:
                    p.mm(ps_cb[:], BCt[b][:, 0, :], BCt[b][:, 1, :])
                    p.tt(cbm[b][:], ps_cb[:], tri[:], ALU.mult)
                for pr in range(2 if LV >= 3 else 0):
                    for jj in range(2):
                        j = pr * 2 + jj
                        h = g * 4 + j
                        pa = ps_a[j % 2]
                        p.mm32(pa[:, 0:128], selh[:, h, :], csT[:])
                        p.ts(dec[j % 2][:], pa[:, 0:128], ncs[:, h:h + 1], 0.0, ALU.add, ALU.min)
                        p.act(dec[j % 2][:], dec[j % 2][:], AF.Exp)
                        p.act(ebc[j % 2][:], pa[:, 0:128], AF.Exp)
                        p.stt(MT[j][:], dec[j % 2][:], dt[:, h:h + 1], cbm[b][:], ALU.mult, ALU.mult)
                        p.tt(CTs[j][:], BCt[b][:, 1, :], ebc[j % 2][:], ALU.mult)
                    if LV < 4:
                        continue
                    for jj in range(2):
                        j = pr * 2 + jj
                        NOOFF = os.environ.get("SSD_NOOFF")
                        if NOOFF == "1":
                            p.mm(ps_y[:, pr * 128:(pr + 1) * 128], xpad[b][:, j, :], MT[j][:], start=(jj == 0), stop=(jj == 1))
                        elif NOOFF == "2":
                            p.mm(ps_y[:, pr * 128:(pr + 1) * 128], STp[:, g * 4 + j, :], CTs[j][:], start=(jj == 0), stop=(jj == 1))
                        else:
                            p.mm(ps_y[:, pr * 128:(pr + 1) * 128], STp[:, g * 4 + j, :], CTs[j][:], start=(jj == 0), stop=False)
                            p.mm(ps_y[:, pr * 128:(pr + 1) * 128], xpad[b][:, j, :], MT[j][:], start=False, stop=(jj == 1))
                    rows = slice(g * 256 + pr * 128, g * 256 + (pr + 1) * 128)
                    if d == 0:
                        p.copy(yo[pr][:], ps_y[:, pr * 128:(pr + 1) * 128], eng="act")
                        p.dma(yfT.ap()[rows, t0:t0 + 128], yo[pr][:], eng="act")
                    else:
                        p.dma(yf[pr][:], yfT.ap()[rows, t0:t0 + 128])
                        p.dma(zt[pr][:], szT.ap()[rows, t0:t0 + 128])
                        p.tt(yo[pr][:], ps_y[:, pr * 128:(pr + 1) * 128], yf[pr][:], ALU.add)
                        p.stt(yo[pr][:], xTt[b][:, pr, :], dsk[:, g * 2 + pr, :], yo[pr][:], ALU.mult, ALU.add)
                        p.tt(yo[pr][:], yo[pr][:], zt[pr][:], ALU.mult, eng="pool")
                        p.dma(ygT.ap()[rows, t0:t0 + 128], yo[pr][:], eng="act")
                if LV < 5:
                    continue
                p.mm(ps_s[:], Btm[b][:], xw[b][:])
                for j in range(4):
                    h = g * 4 + j
                    p.stt(ST32[:, g, j * 64:(j + 1) * 64], ST32[:, g, j * 64:(j + 1) * 64], dl[:, h:h + 1], ps_s[:, j * 64:(j + 1) * 64],
                          ALU.mult, ALU.add)
                    p.copy(STp[:, g * 4 + j, (j % 2) * 64:(j % 2) * 64 + 64], ST32[:, g, j * 64:(j + 1) * 64], eng=("act" if j % 2 else "dve"))
        if d == 0 and os.environ.get("SSD_DBG2"):
            k.dbg = {}
            for nm_, tl, sh, dt_ in (("MT0", MT[0], [128, 128], BF16), ("MT1", MT[1], [128, 128], BF16), ("MT2", MT[2], [128, 128], BF16), ("MT3", MT[3], [128, 128], BF16),
                                     ("dec0", dec[0], [128, 128], F32), ("dec1", dec[1], [128, 128], F32), ("cbm0", cbm[0], [128, 128], F32),
                                     ("xpad0", xpad[0], [128, 512], BF16), ("dt", bufs["dt_tm"][:, 0:32], [128, 32], F32), ("ncs", bufs["ncs_tm"][:, 0:32], [128, 32], F32),
                                     ("xtm0", xtm[0], [128, 256], BF16), ("ncs_snap", ncs_snap, [128, 32], F32), ("la", bufs["la_tm"], [128, 128], F32), ("cs", bufs["cs_tm"], [128, 128], F32), ("yo0", yo[0], [128, 128], F32), ("yo1", yo[1], [128, 128], F32)):
                dd_ = p.dram(k.uid("dbg" + nm_), sh, dt_)
                src_ = tl if not hasattr(tl, "ap") else (tl[:] if len(tl.shape) == 2 else tl[:].rearrange("p a b -> p (a b)"))
                p.dma(dd_.ap(), src_)
                k.dbg[nm_] = dd_
            p.pop()
            return
        if d == 0:
            dST = p.dram(k.uid("dbgST"), [128, G * 256], F32)
            p.dma(dST.ap(), ST32[:].rearrange("p g c -> p (g c)"))
            k.dbg["ST"] = dST
            dxw = p.dram(k.uid("dbgxw"), [128, 256], BF16)
            p.dma(dxw.ap(), xw[1][:])
            k.dbg["xw7"] = dxw
    p.pop()
    if STOP <= 3:
        return
    p.push()
    nw = p.sbuf(k.uid("snw"), [128, 16, 1], F32)
    p.dma(nw[:], W["norm_w"].rearrange("(c p) o -> p c o", p=128))
    yt = [p.sbuf(k.uid("gy"), [128, 2, 512], F32) for _ in range(2)]
    sq = [p.sbuf(k.uid("gsq"), [128, 2, 512], BF16) for _ in range(2)]
    rs = [p.sbuf(k.uid("grs"), [128, 512], F32) for _ in range(2)]
    yn = [p.sbuf(k.uid("gyn"), [128, 2, 512], BF16) for _ in range(2)]
    psn = [p.psum(k.uid("gps"), [128, 512], F32) for _ in range(2)]
    q = 0
    for t0 in range(0, Tt, 512):
        nt_ = min(512, Tt - t0)
        for g in range(G):
            b = q % 2
            q += 1
            p.dma(yt[b][:, :, 0:nt_], ygT.ap()[g * 256:(g + 1) * 256, t0:t0 + nt_].rearrange("(c p) t -> p c t", p=128))
            p.tt(sq[b][:, :, 0:nt_], yt[b][:, :, 0:nt_], yt[b][:, :, 0:nt_], ALU.mult)
            for c in range(2):
                p.mm(psn[b][:, 0:nt_], k.ones16[:], sq[b][:, c, 0:nt_], start=(c == 0), stop=(c == 1))
            p.ts(rs[b][:, 0:nt_], psn[b][:, 0:nt_], 1.0 / 256, EPS, ALU.mult, ALU.add)
            p.act(rs[b][:, 0:nt_], rs[b][:, 0:nt_], AF.Sqrt)
            p.recip(rs[b][:, 0:nt_], rs[b][:, 0:nt_])
            for c in range(2):
                p.stt(yn[b][:, c, 0:nt_], yt[b][:, c, 0:nt_], nw[:, g * 2 + c, :], rs[b][:, 0:nt_], ALU.mult, ALU.mult)
            p.dma(ynT.ap()[g * 256:(g + 1) * 256, t0:t0 + nt_].rearrange("(c p) t -> p c t", p=128), yn[b][:, :, 0:nt_], eng="act")
    p.pop()
    stage_outproj(k, ynT, DI, W["out_w"], h_lat, modT, 0, Tc, Tt)
    if ctx_out:
        stage_outproj(k, ynT, DI, W["out_w"], h_ctx, modT, 1, 0, Tc)


def stage_gdn(k, hT, Tc, T, W, modT, h_lat, h_ctx, ctx_out):
    p = k.P
    Tt = Tc + T
    H, DK, DV = 8, 128, 256
    GDT = F32 if os.environ.get("GDN_F32", "1") == "1" else BF16
    gid = k.ident32 if GDT == F32 else k.ident16
    qkvT = p.dram(k.uid("gdn_qkv"), [4096, Tt], BF16)
    qkvc = p.dram(k.uid("gdn_qkvc"), [4096, Tt], BF16)
    qkn = p.dram(k.uid("gdn_qkn"), [2048, Tt], BF16)
    szT = p.dram(k.uid("gdn_sz"), [2048, Tt], BF16)
    gabT = p.dram(k.uid("gdn_gab"), [32, Tt], F32)
    osum = p.dram(k.uid("gdn_o"), [Tt, 2048], F32)
    ynT = p.dram(k.uid("gdn_yn"), [2048, Tt], BF16)
    p.push()
    e_q = epi_store(k, qkvT, BF16)
    e_z = epi_store(k, szT, BF16, act_fn=AF.Silu, row0=-32 * 128)
    e_g = epi_store(k, gabT, F32, row0=-48 * 128)

    def epi(ps, n, nsz, t0, ntok):
        (e_q if n < 32 else (e_z if n < 48 else e_g))(ps, n, nsz, t0, ntok)
    gemm(k, W["in_w"], hT, Tt, D, 6176, epi)
    p.pop()
    GSTOP = int(os.environ.get("GDN_STOP", "9"))
    if GSTOP <= 1:
        return
    conv_stage(k, qkvT, qkvc, 0, 4096, W["conv_w"], None, Tc, T)
    if GSTOP <= 2:
        return
    p.push()
    xt = [p.sbuf(k.uid("lx"), [128, 512], BF16) for _ in range(2)]
    sq = [p.sbuf(k.uid("lsq"), [128, 512], BF16) for _ in range(2)]
    rs = [p.sbuf(k.uid("lrs"), [128, 512], F32) for _ in range(2)]
    xn = [p.sbuf(k.uid("lxn"), [128, 512], BF16) for _ in range(2)]
    psn = [p.psum(k.uid("lps"), [128, 512], F32) for _ in range(2)]
    q = 0
    for t0 in range(0, Tt, 512):
        nt_ = min(512, Tt - t0)
        for c in range(16):
            b = q % 2
            q += 1
            p.dma(xt[b][:, 0:nt_], qkvc.ap()[c * 128:(c + 1) * 128, t0:t0 + nt_])
            p.tt(sq[b][:, 0:nt_], xt[b][:, 0:nt_], xt[b][:, 0:nt_], ALU.mult, eng="pool")
            p.mm(psn[b][:, 0:nt_], k.ones16[:], sq[b][:, 0:nt_])
            p.ts(rs[b][:, 0:nt_], psn[b][:, 0:nt_], EPS, None, ALU.add)
            p.act(rs[b][:, 0:nt_], rs[b][:, 0:nt_], AF.Sqrt)
            p.recip(rs[b][:, 0:nt_], rs[b][:, 0:nt_])
            p.stt(xn[b][:, 0:nt_], xt[b][:, 0:nt_], (DK ** -0.5 if c < 8 else 1.0), rs[b][:, 0:nt_], ALU.mult, ALU.mult)
            p.dma(qkn.ap()[c * 128:(c + 1) * 128, t0:t0 + nt_], xn[b][:, 0:nt_], eng="act")
    p.pop()
    if GSTOP <= 3:
        return
    p.push()
    C = {n: load_const(k, n) for n in ("tri_f", "tri_b", "nm_f", "nm_b", "sel_f", "sel_b")}
    selh = load_const(k, "selh")
    od = p.sbuf(k.uid("od"), [128, 128], F32)
    stri = p.sbuf(k.uid("stri"), [128, 128], F32)
    p.ts(od[:], k.ident32[:], -1.0, 1.0, ALU.mult, ALU.add)
    bufs = decay_bufs(k)
    S32 = p.sbuf(k.uid("S32"), [128, H, DV], F32)
    S16 = p.sbuf(k.uid("S16"), [128, H, DV], BF16)
    qk = [p.sbuf(k.uid("gqk"), [128, 2, 128], BF16) for _ in range(2)]
    vT = [p.sbuf(k.uid("gvT"), [128, 2, 128], BF16) for _ in range(2)]
    ktm = [p.sbuf(k.uid("gktm"), [128, 128], BF16) for _ in range(2)]
    kw = [p.sbuf(k.uid("gkw"), [128, 128], BF16) for _ in range(2)]
    r32 = [p.sbuf(k.uid("gr32"), [128, 384], F32) for _ in range(2)]
    r16 = [p.sbuf(k.uid("gr16"), [128, 384], GDT) for _ in range(2)]
    dec = [p.sbuf(k.uid("gdec"), [128, 128], F32) for _ in range(2)]
    decod = [p.sbuf(k.uid("gdecod"), [128, 128], F32) for _ in range(2)]
    attnT = [p.sbuf(k.uid("gattn"), [128, 128], BF16) for _ in range(2)]
    Pm = [p.sbuf(k.uid("gP"), [128, 128], GDT) for _ in range(2)]
    Qm = [p.sbuf(k.uid("gQ"), [128, 128], GDT) for _ in range(2)]
    wkT = [p.sbuf(k.uid("gwkT"), [128, 128], BF16) for _ in range(2)]
    wk16 = [p.sbuf(k.uid("gwk16"), [128, 128], BF16) for _ in range(2)]
    vn32 = [p.sbuf(k.uid("gvn32"), [128, DV], F32) for _ in range(2)]
    vn16 = [p.sbuf(k.uid("gvn16"), [128, DV], BF16) for _ in range(2)]
    ob = [p.sbuf(k.uid("gob"), [128, DV], F32) for _ in range(2)]
    o2 = [p.sbuf(k.uid("go2"), [128, DV], F32) for _ in range(2)]
    of = [p.sbuf(k.uid("gof"), [128, DV], F32) for _ in range(2)]
    beta = p.sbuf(k.uid("gbeta"), [128, H], F32)
    nbeta = p.sbuf(k.uid("gnbeta"), [128, H], F32)
    ecs = p.sbuf(k.uid("gecs"), [128, H], F32)
    braw = p.sbuf(k.uid("gbraw"), [128, 128], F32)
    p.memset(braw[:], 0.0)
    ps_tr = p.psum(k.uid("g_tr"), [128, 512], BF16)
    ps_kk_ = p.psum(k.uid("g_kk"), [128, 512], F32)
    ps_kk, ps_a = ps_kk_[:, 0:256], ps_kk_[:, 256:384]
    ps_p = p.psum(k.uid("g_p"), [128, 256], F32)
    ps_r = p.psum(k.uid("g_r"), [128, 384], F32)
    ps_o = p.psum(k.uid("g_o"), [128, 512], F32)
    dtb = p.sbuf(k.uid("gdtb"), [128, 16], F32)
    p.dma(dtb[:], W["dt_bias"].partition_broadcast(128))
    nA = p.sbuf(k.uid("gnA"), [128, 16], F32)
    p.dma(nA[:], W["a_log"].partition_broadcast(128))
    p.act(nA[:], nA[:], AF.Exp)
    p.ts(nA[:], nA[:], -1.0, None, ALU.mult)
    q = 0
    for d in range(2):
        tri = C["tri_f"] if d == 0 else C["tri_b"]
        p.tt(stri[:], tri[:], od[:], ALU.mult)
        p.memset(S32[:], 0.0)
        p.memset(S16[:], 0.0)
        for t0 in chunk_order(Tc, T, d):
            dt, ncs, csT, wx, dl = decay_prep(k, d, H, gabT, d * H, t0, dtb[:, d * H:(d + 1) * H], nA[:, d * H:(d + 1) * H], C, bufs)
            p.dma(braw[0:H, :], gabT.ap()[16 + d * H:16 + (d + 1) * H, t0:t0 + 128])
            p.tr(ps_a[:], braw[:], k.ident32[:])
            p.act(beta[:], ps_a[:, 0:H], AF.Sigmoid)
            p.ts(nbeta[:], beta[:], -1.0, None, ALU.mult)
            p.act(ecs[:], ncs[:, 0:H], AF.Exp, scale=-1.0)
            GLV = int(os.environ.get("GDN_LV", "9"))
            for h in range(H if GLV >= 2 else 0):
                b = q % 2
                q += 1
                p.dma(qk[b][:, 0, :], qkn.ap()[h * 128:(h + 1) * 128, t0:t0 + 128])
                p.dma(qk[b][:, 1, :], qkn.ap()[1024 + h * 128:1024 + (h + 1) * 128, t0:t0 + 128])
                p.dma(vT[b][:], qkvc.ap()[2048 + h * 256:2048 + (h + 1) * 256, t0:t0 + 128].rearrange("(c p) t -> p c t", p=128))
                qT, kT = qk[b][:, 0, :], qk[b][:, 1, :]
                for c in range(2):
                    p.tr(ps_tr[:, c * 128:(c + 1) * 128], vT[b][:, c, :], k.ident16[:])
                p.tr(ps_tr[:, 256:384], kT, k.ident16[:])
                p.copy(ktm[b][:], ps_tr[:, 256:384])
                p.ts(kw[b][:], ktm[b][:], wx[:, h:h + 1], None, ALU.mult)
                p.copy(r32[b][:, 0:256], ps_tr[:, 0:256])
                p.ts(r32[b][:, 256:384], ktm[b][:], ecs[:, h:h + 1], None, ALU.mult)
                p.copy(r16[b][:], r32[b][:], eng="pool")
                if GLV < 3:
                    continue
                p.mm(ps_a[:], selh[:, h, :], csT[:])
                p.ts(dec[b][:], ps_a[:], ncs[:, h:h + 1], 0.0, ALU.add, ALU.min)
                p.act(dec[b][:], dec[b][:], AF.Exp)
                p.tt(decod[b][:], dec[b][:], stri[:], ALU.mult, eng="pool")
                p.tt(dec[b][:], dec[b][:], tri[:], ALU.mult)
                p.mm(ps_kk[:, 0:128], kT, kT)
                p.mm(ps_kk[:, 128:256], kT, qT)
                p.tt(attnT[b][:], ps_kk[:, 128:256], dec[b][:], ALU.mult)
                p.stt(Pm[0][:], ps_kk[:, 0:128], nbeta[:, h:h + 1], decod[b][:], ALU.mult, ALU.mult)
                if GDT == F32:
                    p.tr(ps_p[:, 0:128], Pm[0][:], gid[:])
                    p.copy(Qm[0][:], ps_p[:, 0:128])
                else:
                    p.tr(ps_tr[:, 384:512], Pm[0][:], gid[:])
                    p.copy(Qm[0][:], ps_tr[:, 384:512])
                if GLV < 4:
                    continue
                cur = 0
                for lev in range(7):
                    GS = int(os.environ.get("GDN_SUB", "9"))
                    p.mm(ps_r[:], Pm[cur][:], r16[b][:])
                    if GS >= 2:
                        p.tt(r32[b][:], r32[b][:], ps_r[:], ALU.add)
                    if lev < 6:
                        if GS >= 3:
                            p.copy(r16[b][:], r32[b][:], eng="pool")
                        nx = 1 - cur
                        if GS >= 4:
                            p.mm(ps_p[:, 0:128], Qm[cur][:], Pm[cur][:])
                            p.mm(ps_p[:, 128:256], Pm[cur][:], Qm[cur][:])
                        if GS >= 5:
                            p.copy(Pm[nx][:], ps_p[:, 0:128])
                        if GS >= 6:
                            p.copy(Qm[nx][:], ps_p[:, 128:256])
                        cur = nx
                if GLV < 5:
                    continue
                p.ts(r32[b][:], r32[b][:], beta[:, h:h + 1], None, ALU.mult)
                p.copy(wk16[b][:], r32[b][:, 256:384], eng="act")
                p.tr(ps_tr[:, 384:512], wk16[b][:], k.ident16[:])
                p.copy(wkT[b][:], ps_tr[:, 384:512])
                p.mm(ps_o[:, 0:256], wkT[b][:], S16[:, h, :])
                p.tt(vn32[b][:], r32[b][:, 0:256], ps_o[:, 0:256], ALU.subtract)
                p.copy(vn16[b][:], vn32[b][:], eng="pool")
                if GLV < 6:
                    continue
                p.mm(ps_o[:, 256:512], qT, S16[:, h, :])
                p.copy(o2[b][:], ps_o[:, 256:512])
                p.mm(ps_o[:, 0:256], attnT[b][:], vn16[b][:])
                p.stt(ob[b][:], o2[b][:], ecs[:, h:h + 1], ps_o[:, 0:256], ALU.mult, ALU.add)
                if d == 0:
                    p.dma(osum.ap()[t0:t0 + 128, h * DV:(h + 1) * DV], ob[b][:], eng="act")
                else:
                    p.dma(of[b][:], osum.ap()[t0:t0 + 128, h * DV:(h + 1) * DV])
                    p.tt(ob[b][:], ob[b][:], of[b][:], ALU.add, eng="pool")
                    p.dma(osum.ap()[t0:t0 + 128, h * DV:(h + 1) * DV], ob[b][:], eng="act")
                p.mm(ps_p[:, 0:256], kw[b][:], vn16[b][:])
                p.stt(S32[:, h, :], S32[:, h, :], dl[:, h:h + 1], ps_p[:, 0:256], ALU.mult, ALU.add)
                p.copy(S16[:, h, :], S32[:, h, :], eng="act")
    p.pop()
    if GSTOP <= 4:
        return
    p.push()
    nwb = bc_row(k, "gnw", W["norm_w"], n=DV)
    ot = [p.sbuf(k.uid("fo"), [128, 2048], F32) for _ in range(2)]
    on = [p.sbuf(k.uid("fon"), [128, 2048], BF16) for _ in range(2)]
    ss = [p.sbuf(k.uid("fss"), [128, H], F32) for _ in range(2)]
    jk = p.sbuf(k.uid("fjk"), [128, DV], F32)
    zt = [p.sbuf(k.uid("fz"), [128, 16, 128], BF16) for _ in range(2)]
    yt = [p.sbuf(k.uid("fy"), [128, 16, 128], BF16) for _ in range(2)]
    pst = [p.psum(k.uid("fps"), [128, 1024], BF16) for _ in range(2)]
    for i, t0 in enumerate(range(0, Tt, 128)):
        b = i % 2
        p.dma(ot[b][:], osum.ap()[t0:t0 + 128, :])
        p.dma(zt[b][:], szT.ap()[:, t0:t0 + 128].rearrange("(c p) t -> p c t", p=128), eng="act")
        for h in range(H):
            p.act(jk[:], ot[b][:, h * DV:(h + 1) * DV], AF.Square, accum_out=ss[b][:, h:h + 1])
        p.ts(ss[b][:], ss[b][:], 1.0 / DV, EPS, ALU.mult, ALU.add)
        p.act(ss[b][:], ss[b][:], AF.Sqrt)
        p.recip(ss[b][:], ss[b][:])
        for h in range(H):
            p.stt(on[b][:, h * DV:(h + 1) * DV], ot[b][:, h * DV:(h + 1) * DV], ss[b][:, h:h + 1], nwb[:], ALU.mult, ALU.mult)
        for hh in range(2):
            for c in range(8):
                cc = hh * 8 + c
                p.tr(pst[hh][:, c * 128:(c + 1) * 128], on[b][:, cc * 128:(cc + 1) * 128], k.ident16[:])
            p.copy(yt[b][:, hh * 8:(hh + 1) * 8, :], pst[hh][:].rearrange("p (c t) -> p c t", c=8))
            p.tt(yt[b][:, hh * 8:(hh + 1) * 8, :], yt[b][:, hh * 8:(hh + 1) * 8, :], zt[b][:, hh * 8:(hh + 1) * 8, :], ALU.mult, eng="pool")
        p.dma(ynT.ap()[:, t0:t0 + 128].rearrange("(c p) t -> p c t", p=128), yt[b][:])
    p.pop()
    stage_outproj(k, ynT, 2048, W["out_w"], h_lat, modT, 0, Tc, Tt)
    if ctx_out:
        stage_outproj(k, ynT, 2048, W["out_w"], h_ctx, modT, 1, 0, Tc)


T_LAT, T_CTX, DEPTH_ = 16384, 256, 4
RWKV_SHAPES = {"mu_t": [D, 6], "w_r": [D, D], "w_k": [D, D], "w_v": [D, D], "w_o": [D, D], "w1p": [2, D, 128], "w2p": [2, 128, D], "w0c": [2, D, 1],
               "a1p": [2, D, 128], "a2p": [2, 128, D], "a0c": [2, D, 1], "g1p": [D, 256], "g2p": [256, D], "k_k": [D, 1], "k_a": [D, 1], "r_k": [D, 1],
               "ln_w": [1, D], "ln_b": [1, D]}
MIXERS = {0: "ssd", 1: "gdn", 2: "rwkv", 3: "fnet"}


def build_program(T=T_LAT, Tc=T_CTX, depth=DEPTH_, mixers=MIXERS):
    P = Prog()
    hc = host_consts()
    if "rwkv" in mixers.values():
        hc.update(rwkv_consts())
    if "fnet" in mixers.values():
        hc.update(fnet_consts(T))
    consts = {n: P.dram(n, list(v.shape), F32, kind="ExternalInput") for n, v in hc.items()}
    I = lambda n, sh: P.dram(n, sh, F32, kind="ExternalInput")
    x = I("x", [T, D]); ctx = I("ctx", [Tc, D]); condT = I("condT", [D, 2])
    ada_w = I("ada_w", [depth, D, 6 * D]); ada_b = I("ada_b", [depth, 6 * D])
    n1 = I("norm1_w", [depth, D]); n2 = I("norm2_w", [depth, D]); rw = I("router_w", [depth, D, NE])
    wg = I("expert_w_gate", [depth, NE, D, FF]); wu = I("expert_w_up", [depth, NE, D, FF]); wd = I("expert_w_down", [depth, NE, FF, D])
    fnw = I("final_norm_w", [1, D])
    W = {}
    if "ssd" in mixers.values():
        W["ssd"] = {n: I("ssm_" + n, sh) for n, sh in (("in_w", [D, 6208]), ("conv_w", [4096, 9]), ("conv_b", [4096, 1]), ("dt_bias", [1, 64]),
                                                        ("a_log", [1, 64]), ("d_rep", [2048, 1]), ("norm_w", [2048, 1]), ("out_w", [2048, D]))}
    if "gdn" in mixers.values():
        W["gdn"] = {n: I("gdn_" + n, sh) for n, sh in (("in_w", [D, 6176]), ("conv_w", [4096, 9]), ("dt_bias", [1, 16]), ("a_log", [1, 16]),
                                                        ("norm_w", [1, 256]), ("out_w", [2048, D]))}
    if "rwkv" in mixers.values():
        W["rwkv"] = {n: I("rwkv_" + n, sh) for n, sh in RWKV_SHAPES.items()}
    if "fnet" in mixers.values():
        W["fnet"] = {"out_w": I("fnet_out_w", [D, D])}
    out = P.dram("out", [T, D], F32, kind="ExternalOutput")
    k = K(P, consts)
    h_lat = P.dram("h_lat", [T, D], F32)
    h_ctx = P.dram("h_ctx", [Tc, D], F32)
    for r0 in range(0, T, 256):
        P.dma(h_lat.ap()[r0:r0 + 256, :], x.ap()[r0:r0 + 256, :], eng=("sp" if (r0 // 256) % 2 == 0 else "act"))
    P.dma(h_ctx.ap(), ctx.ap(), eng="act")
    hT = P.dram("hT", [D, Tc + T], BF16)
    for i in range(depth):
        m = i % 4
        ctx_out = any((j % 4) in (0, 1, 2) for j in range(i + 1, depth))
        ctx_in = ctx_out or (m in (0, 1, 2))
        modT = P.dram(f"modT{i}", [2, 6 * D], F32)
        stage_ada(k, condT, ada_w.ap()[i], ada_b.ap()[i:i + 1, :], modT)
        mx = mixers.get(m)
        if mx is not None:
            P.push()
            g_bc, sh_bc = mod_tiles(k, n1.ap()[i:i + 1, :], modT, 0, 0, 1)
            stage_norm(k, h_lat, T, g_bc, sh_bc, outT16=hT, col0=Tc)
            P.pop()
            if ctx_in:
                P.push()
                g_bc, sh_bc = mod_tiles(k, n1.ap()[i:i + 1, :], modT, 1, 0, 1)
                stage_norm(k, h_ctx, Tc, g_bc, sh_bc, outT16=hT, col0=0)
                P.pop()
            Wm = {n: v.ap() for n, v in W[mx].items()}
            if mx == "rwkv":
                Wm = {n: ([v.ap()[0], v.ap()[1]] if n in ("w1p", "w2p", "w0c", "a1p", "a2p", "a0c") else v.ap()) for n, v in W[mx].items()}
            if mx == "fnet":
                stage_fnet(k, hT, Tc, T, Wm, modT, h_lat)
            else:
                {"ssd": stage_ssd, "gdn": stage_gdn, "rwkv": stage_rwkv}[mx](k, hT, Tc, T, Wm, modT, h_lat, h_ctx, ctx_out)
        eg = lambda a: [a.ap()[i, e] for e in range(NE)]
        if os.environ.get("KSKIP_LAST_MOE") and i == depth - 1:
            continue
        stage_moe(k, h_lat, T, T * 2 // NE, n2.ap()[i:i + 1, :], modT, 0, rw.ap()[i], eg(wg), eg(wu), eg(wd), consts["tokid"])
        if ctx_out:
            stage_moe(k, h_ctx, Tc, Tc * 2 // NE, n2.ap()[i:i + 1, :], modT, 1, rw.ap()[i], eg(wg), eg(wu), eg(wd), consts["tokid"])
    P.push()
    g_bc = bc_row(k, "fg", fnw.ap())
    z_bc = P.sbuf("fz", [128, D], F32)
    P.memset(z_bc[:], 0.0)
    stage_norm(k, h_lat, T, g_bc, z_bc, out_tm32=out)
    P.pop()
    return P.finish(), hc, P


_CACHE = {}


def kernel(**inp):
    f32 = lambda a: np.ascontiguousarray(np.asarray(a, dtype=np.float32))
    mixers = MIXERS
    if "prog" not in _CACHE:
        _CACHE["prog"] = build_program(mixers=mixers)
    nc, hc, _ = _CACHE["prog"]
    B = inp["x"].shape[0]
    shared = dict(hc)
    for n in ("ada_w", "ada_b", "norm1_w", "norm2_w", "router_w", "expert_w_gate", "expert_w_up", "expert_w_down"):
        shared[n] = f32(inp[n])
    shared["final_norm_w"] = f32(inp["final_norm_w"]).reshape(1, D)
    if "ssd" in mixers.values():
        shared.update({"ssm_in_w": f32(inp["ssm_in_w"][0]), "ssm_conv_w": f32(np.asarray(inp["ssm_conv_w"][0]).reshape(9, 4096).T),
                       "ssm_conv_b": f32(inp["ssm_conv_b"][0]).reshape(4096, 1), "ssm_dt_bias": f32(inp["ssm_dt_bias"][0]).reshape(1, 64),
                       "ssm_a_log": f32(inp["ssm_a_log"][0]).reshape(1, 64), "ssm_d_rep": f32(np.repeat(np.asarray(inp["ssm_d"][0]), 64)).reshape(2048, 1),
                       "ssm_norm_w": f32(inp["ssm_norm_w"][0]).reshape(2048, 1), "ssm_out_w": f32(inp["ssm_out_w"][0])})
    if "gdn" in mixers.values():
        shared.update({"gdn_in_w": f32(inp["gdn_in_w"][0]), "gdn_conv_w": f32(np.asarray(inp["gdn_conv_w"][0]).reshape(9, 4096).T),
                       "gdn_dt_bias": f32(inp["gdn_dt_bias"][0]).reshape(1, 16), "gdn_a_log": f32(inp["gdn_a_log"][0]).reshape(1, 16),
                       "gdn_norm_w": f32(inp["gdn_norm_w"][0]).reshape(1, 256), "gdn_out_w": f32(inp["gdn_out_w"][0])})
    if "rwkv" in mixers.values():
        pad = lambda a, sh: np.ascontiguousarray(np.pad(np.asarray(a, dtype=np.float32), [(0, s_ - d_) for s_, d_ in zip(sh, np.shape(a))]))
        g = lambda n: np.asarray(inp["rwkv_" + n][0], dtype=np.float32)
        shared.update({"rwkv_mu_t": f32(g("mu").T), "rwkv_w_r": f32(g("w_r")), "rwkv_w_k": f32(g("w_k")), "rwkv_w_v": f32(g("w_v")), "rwkv_w_o": f32(g("w_o")),
                       "rwkv_w1p": pad(g("w1"), (2, D, 128)), "rwkv_w2p": pad(g("w2"), (2, 128, D)), "rwkv_w0c": f32(g("w0")[:, :, None]),
                       "rwkv_a1p": pad(g("a1"), (2, D, 128)), "rwkv_a2p": pad(g("a2"), (2, 128, D)), "rwkv_a0c": f32(g("a0")[:, :, None]),
                       "rwkv_g1p": pad(g("g1"), (D, 256)), "rwkv_g2p": pad(g("g2"), (256, D)), "rwkv_k_k": f32(g("k_k")[:, None]),
                       "rwkv_k_a": f32(g("k_a")[:, None]), "rwkv_r_k": f32(g("r_k").reshape(D, 1)), "rwkv_ln_w": f32(g("ln_w")[None]), "rwkv_ln_b": f32(g("ln_b")[None])})
    if "fnet" in mixers.values():
        shared["fnet_out_w"] = f32(inp["fnet_out_w"][0])
    in_maps = []
    for b in range(B):
        m = dict(shared)
        m["x"] = f32(inp["x"][b]); m["ctx"] = f32(inp["ctx"][b])
        m["condT"] = f32(np.stack([np.asarray(inp["c"][b]), np.asarray(inp["c_ctx"])], axis=1))
        in_maps.append(m)
    res = run_bass_kernel_spmd(nc, in_maps, core_ids=list(range(B)))
    return np.stack([np.asarray(r["out"], dtype=np.float32) for r in res.results], axis=0)


def fnet_consts(T):
    T1 = T // 128
    C = 256
    c = np.arange(C)
    ang = 2 * np.pi * np.outer(c, c) / C
    wch = np.zeros((1024, 2048), np.float32)
    for g in range(4):
        wch[g * C:(g + 1) * C, g * C:(g + 1) * C] = np.cos(ang)
        wch[g * C:(g + 1) * C, 1024 + g * C:1024 + (g + 1) * C] = -np.sin(ang)
    a1 = 2 * np.pi * np.outer(np.arange(T1), np.arange(T1)) / T1
    fa_r = np.concatenate([np.cos(a1), -np.sin(a1)], 1).astype(np.float32)
    fa_i = np.concatenate([np.sin(a1), np.cos(a1)], 1).astype(np.float32)
    tw = 2 * np.pi * np.outer(np.arange(128), np.arange(T1)) / T
    sc = 1.0 / np.sqrt(T * C)
    a2 = 2 * np.pi * np.outer(np.arange(128), np.arange(128)) / 128
    return {"f_wch": wch, "f_fa_r": fa_r, "f_fa_i": fa_i, "f_twc": (np.cos(tw) * sc).astype(np.float32), "f_tws": (np.sin(tw) * sc).astype(np.float32),
            "f_c128": np.cos(a2).astype(np.float32), "f_s128": np.sin(a2).astype(np.float32)}


def stage_fnet(k, hT, Tc, T, W, modT, h_lat):
    p = k.P
    T1 = T // 128
    UT = p.dram(k.uid("fn_U"), [2048, T], BF16)
    yT = p.dram(k.uid("fn_y"), [D, T], BF16)
    hTl = p.dram(k.uid("fn_h"), [D, T], BF16)
    for r0 in range(0, D, 128):
        p.dma(hTl.ap()[r0:r0 + 128, :], hT.ap()[r0:r0 + 128, Tc:Tc + T], eng=("sp" if (r0 // 128) % 2 == 0 else "act"))
    p.push()
    gemm(k, k.c["f_wch"].ap(), hTl, T, D, 2048, epi_store(k, UT, BF16))
    p.pop()
    p.push()
    fa_r = load_const(k, "f_fa_r", dt=BF16)
    fa_i = load_const(k, "f_fa_i", dt=BF16)
    c128 = load_const(k, "f_c128", dt=BF16)
    s128 = load_const(k, "f_s128", dt=BF16)
    twc = load_const(k, "f_twc")
    tws = load_const(k, "f_tws")
    NB = 4
    ur = [p.sbuf(k.uid("fur"), [T1, NB, 128], BF16) for _ in range(2)]
    ui = [p.sbuf(k.uid("fui"), [T1, NB, 128], BF16) for _ in range(2)]
    qq = [p.sbuf(k.uid("fq"), [128, 2, NB, T1], BF16) for _ in range(2)]
    ta = [p.sbuf(k.uid("fta"), [128, 2, T1], F32) for _ in range(2)]
    tb = [p.sbuf(k.uid("ftb"), [128, 2, T1], F32) for _ in range(2)]
    yo = [p.sbuf(k.uid("fyo"), [128, NB, T1], BF16) for _ in range(2)]
    psA = [p.psum(k.uid("fpa"), [128, 2 * T1], F32) for _ in range(2)]
    psC = [p.psum(k.uid("fpc"), [128, NB * T1], F32) for _ in range(2)]
    for i, c0 in enumerate(range(0, D, NB)):
        b = i % 2
        p.dma(ur[b][:], UT.ap()[c0:c0 + NB, :].rearrange("c (a t) -> a c t", t=128))
        p.dma(ui[b][:], UT.ap()[1024 + c0:1024 + c0 + NB, :].rearrange("c (a t) -> a c t", t=128), eng="act")
        for j in range(NB):
            pa = psA[j % 2]
            p.mm(pa[:], ur[b][:, j, :], fa_r[:], start=True, stop=False)
            p.mm(pa[:], ui[b][:, j, :], fa_i[:], start=False, stop=True)
            P2 = pa[:].rearrange("p (r t) -> p r t", r=2)
            a_, b_ = ta[j % 2], tb[j % 2]
            p.tt(a_[:], P2, twc[:].unsqueeze(1).to_broadcast([128, 2, T1]), ALU.mult)
            p.tt(b_[:], P2, tws[:].unsqueeze(1).to_broadcast([128, 2, T1]), ALU.mult)
            p.tt(qq[b][:, 0, j, :], a_[:, 0, :], b_[:, 1, :], ALU.add, eng="pool")
            p.tt(qq[b][:, 1, j, :], a_[:, 1, :], b_[:, 0, :], ALU.subtract, eng="pool")
        pc = psC[b]
        p.mm(pc[:], c128[:], qq[b][:, 0, :, :].rearrange("p c t -> p (c t)"), start=True, stop=False)
        p.mm(pc[:], s128[:], qq[b][:, 1, :, :].rearrange("p c t -> p (c t)"), start=False, stop=True)
        p.copy(yo[b][:], pc[:].rearrange("p (c t) -> p c t", c=NB))
        p.dma(yT.ap()[c0:c0 + NB, :].rearrange("c (a t) -> a c t", t=T1), yo[b][:], eng="act")
    p.pop()
    stage_outproj(k, yT, D, W["out_w"], h_lat, modT, 0, 0, T)


def rwkv_consts():
    i = np.arange(128)
    blk = ((i[:, None] // 64) == (i[None, :] // 64)).astype(np.float32)
    tri_f = (i[:, None] <= i[None, :]).astype(np.float32)
    tri_b = (i[:, None] >= i[None, :]).astype(np.float32)
    off = 1.0 - np.eye(128, dtype=np.float32)
    return {"r_blk64": blk, "r_m_f": np.concatenate([tri_f * off, tri_f], 1), "r_m_b": np.concatenate([tri_b * off, tri_b], 1)}


def epi_bias_act(k, outT, dt, func, bias_col=None, scale=None, post_mul=None):
    p = k.P
    ob = [p.sbuf(k.uid("eb"), [128, 512], dt) for _ in range(3)]
    st = {"i": 0}

    def epi(ps, n, nsz, t0, ntok):
        o = ob[st["i"] % 3]
        st["i"] += 1
        kw = {}
        if bias_col is not None:
            kw["bias"] = bias_col[0:nsz, n, :]
        p.act(o[0:nsz, 0:ntok], ps, func, **kw)
        if post_mul is not None:
            p.ts(o[0:nsz, 0:ntok], o[0:nsz, 0:ntok], post_mul, None, ALU.mult, eng="pool")
        p.dma(outT.ap()[n * 128:n * 128 + nsz, t0:t0 + ntok], o[0:nsz, 0:ntok], eng="act")
    return epi


def stage_rwkv(k, hT, Tc, T, W, modT, h_lat, h_ctx, ctx_out):
    p = k.P
    Tt = Tc + T
    NH, HD = 16, 64
    mk = lambda nm, rows=D, dt=BF16: p.dram(k.uid("rw_" + nm), [rows, Tt], dt)
    xm = [mk(f"x{q}") for q in range(6)]
    rT, kT, vT, gT, aT, bonT = mk("r"), mk("k"), mk("v"), mk("g"), mk("a"), mk("bon")
    t1 = [mk(f"t1{d}", 128) for d in range(2)]
    t2 = [mk(f"t2{d}", 128) for d in range(2)]
    t3 = mk("t3", 256)
    lwT = [mk(f"lw{d}", D, F32) for d in range(2)]
    arT = [mk(f"ar{d}") for d in range(2)]
    bT = [mk(f"b{d}") for d in range(2)]
    kdT = [mk(f"kd{d}") for d in range(2)]
    ysum = p.dram(k.uid("rw_y"), [Tt, D], F32)
    ynT = mk("yn")
    seqs = ([(0, Tc)] if Tc > 0 else []) + [(Tc, Tt)]
    p.push()
    mu = p.sbuf(k.uid("mu"), [128, NCH, 6], F32)
    p.dma(mu[:], W["mu_t"].rearrange("(c p) q -> p c q", p=128))
    PIECE = 2048
    hb = [p.sbuf(k.uid("mh"), [128, PIECE + 2], BF16) for _ in range(2)]
    xx = [p.sbuf(k.uid("mxx"), [128, PIECE], F32) for _ in range(2)]
    ob = [p.sbuf(k.uid("mo"), [128, PIECE], BF16) for _ in range(3)]
    i = 0
    oi = 0
    for (lo, hi) in seqs:
        for c in range(NCH):
            for t0 in range(lo, hi, PIECE):
                n = min(PIECE, hi - t0)
                b = i % 2
                i += 1
                left, right = (t0 - 1 >= lo), (t0 + n < hi)
                if not left:
                    p.memset(hb[b][:, 0:1], 0.0)
                if not right:
                    p.memset(hb[b][:, n + 1:n + 2], 0.0)
                p.dma(hb[b][:, (0 if left else 1):n + 1 + (1 if right else 0)],
                      hT.ap()[c * 128:(c + 1) * 128, t0 - (1 if left else 0):t0 + n + (1 if right else 0)])
                p.tt(xx[b][:, 0:n], hb[b][:, 0:n], hb[b][:, 2:n + 2], ALU.add)
                p.stt(xx[b][:, 0:n], xx[b][:, 0:n], 0.5, hb[b][:, 1:n + 1], ALU.mult, ALU.subtract)
                for q in range(6):
                    o = ob[oi % 3]
                    oi += 1
                    p.stt(o[:, 0:n], xx[b][:, 0:n], mu[:, c, q:q + 1], hb[b][:, 1:n + 1], ALU.mult, ALU.add)
                    p.dma(xm[q].ap()[c * 128:(c + 1) * 128, t0:t0 + n], o[:, 0:n], eng="act")
    p.pop()
    def col(nm, ap):
        t = p.sbuf(k.uid(nm), [128, NCH, 1], F32)
        p.dma(t[:], ap.rearrange("(c p) o -> p c o", p=128))
        return t
    for (wn, xq, dst) in (("w_r", 0, rT), ("w_k", 2, kT), ("w_v", 3, vT)):
        p.push()
        gemm(k, W[wn], xm[xq], Tt, D, D, epi_store(k, dst, BF16))
        p.pop()
    for d in range(2):
        p.push()
        gemm(k, W["w1p"][d], xm[1], Tt, D, 128, epi_store(k, t1[d], BF16, act_fn=AF.Tanh))
        p.pop()
        p.push()
        w0c = col("w0c", W["w0c"][d])
        gemm(k, W["w2p"][d], t1[d], Tt, 128, D, epi_bias_act(k, lwT[d], F32, AF.Sigmoid, bias_col=w0c, post_mul=-math.exp(-0.5)))
        p.pop()
        p.push()
        gemm(k, W["a1p"][d], xm[4], Tt, D, 128, epi_store(k, t2[d], BF16))
        p.pop()
        p.push()
        a0c = col("a0c", W["a0c"][d])
        gemm(k, W["a2p"][d], t2[d], Tt, 128, D, epi_bias_act(k, arT[d], BF16, AF.Sigmoid, bias_col=a0c))
        p.pop()
    p.push()
    gemm(k, W["g1p"], xm[5], Tt, D, 256, epi_store(k, t3, BF16, act_fn=AF.Sigmoid))
    p.pop()
    p.push()
    gemm(k, W["g2p"], t3, Tt, 256, D, epi_store(k, gT, BF16))
    p.pop()
    p.push()
    blk = load_const(k, "r_blk64", dt=BF16)
    kkc, kac, rkc = col("kkc", W["k_k"]), col("kac", W["k_a"]), col("rkc", W["r_k"])
    tk_ = [p.sbuf(k.uid("ek"), [128, 512], BF16) for _ in range(2)]
    tr_ = [p.sbuf(k.uid("er"), [128, 512], BF16) for _ in range(2)]
    tv_ = [p.sbuf(k.uid("ev"), [128, 512], BF16) for _ in range(2)]
    ta_ = [[p.sbuf(k.uid("ea"), [128, 512], BF16) for _ in range(2)] for _ in range(2)]
    kq = p.sbuf(k.uid("ekq"), [128, 512], F32)
    sq = p.sbuf(k.uid("esq"), [128, 512], BF16)
    rs = p.sbuf(k.uid("ers"), [128, 512], F32)
    kkt = p.sbuf(k.uid("ekk"), [128, 512], F32)
    f1 = p.sbuf(k.uid("ef1"), [128, 512], F32)
    kds = p.sbuf(k.uid("ekds"), [128, 512], F32)
    o16 = [p.sbuf(k.uid("eo"), [128, 512], BF16) for _ in range(4)]
    psn = [p.psum(k.uid("eps"), [128, 512], F32) for _ in range(2)]
    oi = 0
    qi = 0
    for t0 in range(0, Tt, 512):
        n = min(512, Tt - t0)
        for c in range(NCH):
            b = qi % 2
            qi += 1
            rows = slice(c * 128, (c + 1) * 128)
            p.dma(tk_[b][:, 0:n], kT.ap()[rows, t0:t0 + n])
            p.dma(tr_[b][:, 0:n], rT.ap()[rows, t0:t0 + n])
            p.dma(tv_[b][:, 0:n], vT.ap()[rows, t0:t0 + n], eng="act")
            for d in range(2):
                p.dma(ta_[d][b][:, 0:n], arT[d].ap()[rows, t0:t0 + n], eng="act")
            p.ts(kq[:, 0:n], tk_[b][:, 0:n], kkc[:, c, :], None, ALU.mult)
            p.tt(sq[:, 0:n], kq[:, 0:n], kq[:, 0:n], ALU.mult, eng="pool")
            p.mm(psn[0][:, 0:n], blk[:], sq[:, 0:n])
            p.ts(rs[:, 0:n], psn[0][:, 0:n], EPS, None, ALU.add)
            p.act(rs[:, 0:n], rs[:, 0:n], AF.Sqrt)
            p.recip(rs[:, 0:n], rs[:, 0:n])
            p.tt(kkt[:, 0:n], kq[:, 0:n], rs[:, 0:n], ALU.mult)
            o = o16[oi % 4]; oi += 1
            p.ts(o[:, 0:n], kkt[:, 0:n], -1.0, None, ALU.mult, eng="pool")
            p.dma(aT.ap()[rows, t0:t0 + n], o[:, 0:n], eng="act")
            for d in range(2):
                o = o16[oi % 4]; oi += 1
                p.tt(o[:, 0:n], kkt[:, 0:n], ta_[d][b][:, 0:n], ALU.mult)
                p.dma(bT[d].ap()[rows, t0:t0 + n], o[:, 0:n], eng="act")
                p.ts(f1[:, 0:n], ta_[d][b][:, 0:n], -1.0, None, ALU.add, eng="pool")
                p.ts(f1[:, 0:n], f1[:, 0:n], kac[:, c, :], None, ALU.mult)
                p.stt(f1[:, 0:n], f1[:, 0:n], 1.0, tk_[b][:, 0:n], ALU.add, ALU.mult)
                o = o16[oi % 4]; oi += 1
                p.copy(o[:, 0:n], f1[:, 0:n], eng="pool")
                p.dma(kdT[d].ap()[rows, t0:t0 + n], o[:, 0:n], eng="act")
                if d == 0:
                    p.copy(kds[:, 0:n], f1[:, 0:n])
                else:
                    p.tt(kds[:, 0:n], kds[:, 0:n], f1[:, 0:n], ALU.add)
            p.stt(sq[:, 0:n], kds[:, 0:n], rkc[:, c, :], tr_[b][:, 0:n], ALU.mult, ALU.mult)
            p.mm(psn[1][:, 0:n], blk[:], sq[:, 0:n])
            o = o16[oi % 4]; oi += 1
            p.tt(o[:, 0:n], psn[1][:, 0:n], tv_[b][:, 0:n], ALU.mult)
            p.dma(bonT.ap()[rows, t0:t0 + n], o[:, 0:n], eng="act")
    p.pop()
    p.push()
    rc = {n_: load_const(k, n_) for n_ in ("r_m_f", "r_m_b")}
    S32 = p.sbuf(k.uid("rS32"), [128, 8, HD], F32)
    S16 = p.sbuf(k.uid("rS16"), [128, 8, HD], BF16)
    ones_ = p.sbuf(k.uid("rones"), [128, 128], F32)
    p.memset(ones_[:], 1.0)
    ld = {n_: [p.sbuf(k.uid("l" + n_), [128, 128], BF16) for _ in range(2)] for n_ in ("r", "v", "a", "kd", "b")}
    lwt = [p.sbuf(k.uid("llw"), [128, 128], F32) for _ in range(2)]
    cw = p.sbuf(k.uid("rcw"), [128, 128], F32)
    tmp = p.sbuf(k.uid("rtmp"), [128, 128], F32)
    ecw = p.sbuf(k.uid("recw"), [128, 128], F32)
    encw = p.sbuf(k.uid("rencw"), [128, 128], F32)
    ecwm = p.sbuf(k.uid("recwm"), [128, 128], F32)
    AR = [p.sbuf(k.uid("rAR"), [128, 256], BF16) for _ in range(2)]
    for t_ in AR:
        p.memset(t_[:], 0.0)
    bt = p.sbuf(k.uid("rbt"), [128, 128], BF16)
    kt = p.sbuf(k.uid("rkt"), [128, 128], BF16)
    btm = [p.sbuf(k.uid("rbtm"), [128, 128], BF16) for _ in range(2)]
    ktm = [p.sbuf(k.uid("rktm"), [128, 128], BF16) for _ in range(2)]
    for t_ in btm + ktm:
        p.memset(t_[:], 0.0)
    vtm = p.sbuf(k.uid("rvtm"), [128, 128], BF16)
    Mm = [p.sbuf(k.uid("rM"), [128, 256], BF16) for _ in range(2)]
    Pm = [p.sbuf(k.uid("rP"), [128, 128], F32) for _ in range(2)]
    Qm = [p.sbuf(k.uid("rQ"), [128, 128], F32) for _ in range(2)]
    u32 = p.sbuf(k.uid("ru32"), [128, HD], F32)
    u16 = [p.sbuf(k.uid("ru16"), [128, HD], BF16) for _ in range(2)]
    yb = [p.sbuf(k.uid("ryb"), [128, 128], F32) for _ in range(2)]
    yf = [p.sbuf(k.uid("ryf"), [128, 128], F32) for _ in range(2)]
    ps_tr = p.psum(k.uid("r_tr"), [128, 512], BF16)
    ps_m = p.psum(k.uid("r_m"), [128, 512], F32)
    ps_p = p.psum(k.uid("r_p"), [128, 256], F32)
    ps_u = p.psum(k.uid("r_u"), [128, 128], F32)
    ps_y = p.psum(k.uid("r_y"), [128, 128], F32)
    ps_s = p.psum(k.uid("r_s"), [128, HD], F32)
    qi = 0
    for d in range(2):
        msk = rc["r_m_f"] if d == 0 else rc["r_m_b"]
        ex = 127 if d == 0 else 0
        p.memset(S32[:], 0.0)
        p.memset(S16[:], 0.0)
        for t0 in chunk_order(Tc, T, d):
            for pr in range(8):
                b = qi % 2
                qi += 1
                rows = slice(pr * 128, (pr + 1) * 128)
                for n_, src in (("r", rT), ("v", vT), ("a", aT), ("kd", kdT[d]), ("b", bT[d])):
                    p.dma(ld[n_][b][:], src.ap()[rows, t0:t0 + 128], eng=("sp" if n_ in ("r", "a", "b") else "act"))
                p.dma(lwt[b][:], lwT[d].ap()[rows, t0:t0 + 128])
                p.op("dve", lambda e, b=b: e.tensor_tensor_scan(cw[:], ones_[:], lwt[b][:], 0.0, ALU.mult, ALU.add),
                     reads=[ones_, lwt[b]], writes=[cw])
                if d == 1:
                    p.ts(tmp[:], cw[:], -1.0, cw[:, 127:128], ALU.mult, ALU.add)
                    p.tt(cw[:], tmp[:], lwt[b][:], ALU.add)
                p.act(ecw[:], cw[:], AF.Exp)
                p.act(encw[:], cw[:], AF.Exp, scale=-1.0)
                p.tt(tmp[:], cw[:], lwt[b][:], ALU.subtract)
                p.act(ecwm[:], tmp[:], AF.Exp)
                p.tt(bt[:], ld["b"][b][:], encw[:], ALU.mult)
                p.tt(kt[:], ld["kd"][b][:], encw[:], ALU.mult)
                for j in range(2):
                    hs = slice(j * 64, (j + 1) * 64)
                    p.tt(AR[j][hs, 0:128], ld["a"][b][hs, :], ecwm[hs, :], ALU.mult)
                    p.tt(AR[j][hs, 128:256], ld["r"][b][hs, :], ecw[hs, :], ALU.mult)
                p.tr(ps_tr[:, 0:128], bt[:], k.ident16[:])
                p.tr(ps_tr[:, 128:256], kt[:], k.ident16[:])
                p.tr(ps_tr[:, 256:384], ld["v"][b][:], k.ident16[:])
                for j in range(2):
                    cs_ = slice(j * 64, (j + 1) * 64)
                    p.copy(btm[j][:, cs_], ps_tr[:, j * 64:(j + 1) * 64])
                    p.copy(ktm[j][:, cs_], ps_tr[:, 128 + j * 64:128 + (j + 1) * 64])
                p.copy(vtm[:], ps_tr[:, 256:384])
                for j in range(2):
                    h = pr * 2 + j
                    vj = vtm[:, j * 64:(j + 1) * 64]
                    p.mm(ps_m[:, 0:256], bt[:], AR[j][:])
                    p.mm(ps_m[:, 256:512], kt[:], AR[j][:])
                    p.tt(Mm[0][:], ps_m[:, 0:256], msk[:], ALU.mult)
                    p.tt(Mm[1][:], ps_m[:, 256:512], msk[:], ALU.mult)
                    p.mm(ps_u[:, 0:HD], AR[j][:, 0:128], S16[:, pr, :], start=True, stop=False)
                    p.mm(ps_u[:, 0:HD], Mm[1][:, 0:128], vj, start=False, stop=True)
                    p.copy(u32[:], ps_u[:, 0:HD])
                    p.copy(u16[0][:], u32[:], eng="pool")
                    p.tt(Pm[0][:], ps_m[:, 0:128], msk[:, 0:128], ALU.mult)
                    p.tr(ps_p[:, 0:128], Pm[0][:], k.ident32[:])
                    p.copy(Qm[0][:], ps_p[:, 0:128])
                    cur = 0
                    for lev in range(7):
                        p.mm(ps_u[:, 64:64 + HD], Pm[cur][:], u32[:])
                        p.tt(u32[:], u32[:], ps_u[:, 64:64 + HD], ALU.add)
                        if lev == 6:
                            p.copy(u16[0][:], u32[:], eng="pool")
                        if lev < 6:
                            nx = 1 - cur
                            p.mm(ps_p[:, 0:128], Qm[cur][:], Pm[cur][:])
                            p.mm(ps_p[:, 128:256], Pm[cur][:], Qm[cur][:])
                            p.copy(Pm[nx][:], ps_p[:, 0:128])
                            p.copy(Qm[nx][:], ps_p[:, 128:256])
                            cur = nx
                    yp = ps_y[:, j * 64:(j + 1) * 64]
                    p.mm(yp, AR[j][:, 128:256], S16[:, pr, :], start=True, stop=False)
                    p.mm(yp, Mm[0][:, 128:256], u16[0][:], start=False, stop=False)
                    p.mm(yp, Mm[1][:, 128:256], vj, start=False, stop=True)
                    p.mm(ps_s[:], btm[j][:], u16[0][:], start=(j == 0), stop=False)
                    p.mm(ps_s[:], ktm[j][:], vj, start=False, stop=(j == 1))
                if d == 0:
                    p.copy(yb[b][:], ps_y[:])
                    p.dma(ysum.ap()[t0:t0 + 128, rows], yb[b][:], eng="act")
                else:
                    p.dma(yf[b][:], ysum.ap()[t0:t0 + 128, rows])
                    p.tt(yb[b][:], ps_y[:], yf[b][:], ALU.add)
                    p.dma(ysum.ap()[t0:t0 + 128, rows], yb[b][:], eng="act")
                p.tt(S32[:, pr, :], S32[:, pr, :], ps_s[:], ALU.add)
                p.ts(S32[:, pr, :], S32[:, pr, :], ecw[:, ex:ex + 1], None, ALU.mult)
                p.copy(S16[:, pr, :], S32[:, pr, :], eng="pool")
    p.pop()
    p.push()
    lnw = bc_row(k, "lnw", W["ln_w"])
    lnb = bc_row(k, "lnb", W["ln_b"])
    GE = 64e-5
    yt = [p.sbuf(k.uid("zy"), [128, D], F32) for _ in range(2)]
    sqt = p.sbuf(k.uid("zsq"), [128, D], F32)
    mean = p.sbuf(k.uid("zmean"), [128, NH], F32)
    ex2 = p.sbuf(k.uid("zex2"), [128, NH], F32)
    var = p.sbuf(k.uid("zvar"), [128, NH], F32)
    y16 = [p.sbuf(k.uid("zy16"), [128, D], BF16) for _ in range(2)]
    bo = [p.sbuf(k.uid("zbo"), [128, NCH, 128], BF16) for _ in range(2)]
    gt = [p.sbuf(k.uid("zg"), [128, NCH, 128], BF16) for _ in range(2)]
    yo = [p.sbuf(k.uid("zyo"), [128, NCH, 128], BF16) for _ in range(2)]
    pst = p.psum(k.uid("zps"), [128, D], BF16)
    for i, t0 in enumerate(range(0, Tt, 128)):
        b = i % 2
        p.dma(yt[b][:], ysum.ap()[t0:t0 + 128, :])
        p.dma(bo[b][:], bonT.ap()[:, t0:t0 + 128].rearrange("(c p) t -> p c t", p=128), eng="act")
        p.dma(gt[b][:], gT.ap()[:, t0:t0 + 128].rearrange("(c p) t -> p c t", p=128), eng="act")
        Y3 = yt[b][:].rearrange("p (h n) -> p h n", h=NH)
        p.op("dve", lambda e, Y3=Y3: e.tensor_reduce(mean[:], Y3, AX.X, ALU.add), reads=[yt[b]], writes=[mean])
        p.tt(sqt[:], yt[b][:], yt[b][:], ALU.mult, eng="pool")
        S3 = sqt[:].rearrange("p (h n) -> p h n", h=NH)
        p.op("dve", lambda e, S3=S3: e.tensor_reduce(ex2[:], S3, AX.X, ALU.add), reads=[sqt], writes=[ex2])
        p.ts(mean[:], mean[:], 1.0 / HD, None, ALU.mult)
        p.tt(var[:], mean[:], mean[:], ALU.mult)
        p.stt(var[:], ex2[:], 1.0 / HD, var[:], ALU.mult, ALU.subtract)
        p.ts(var[:], var[:], GE, None, ALU.add)
        p.act(var[:], var[:], AF.Sqrt)
        p.recip(var[:], var[:])
        p.tt(Y3, Y3, mean[:].unsqueeze(2).to_broadcast([128, NH, HD]), ALU.subtract)
        p.tt(Y3, Y3, var[:].unsqueeze(2).to_broadcast([128, NH, HD]), ALU.mult)
        p.tt(yt[b][:], yt[b][:], lnw[:], ALU.mult, eng="pool")
        p.tt(y16[b][:], yt[b][:], lnb[:], ALU.add)
        for c in range(NCH):
            p.tr(pst[:, c * 128:(c + 1) * 128], y16[b][:, c * 128:(c + 1) * 128], k.ident16[:])
        p.copy(yo[b][:], pst[:].rearrange("p (c t) -> p c t", c=NCH))
        p.tt(yo[b][:], yo[b][:], bo[b][:], ALU.add, eng="pool")
        p.tt(yo[b][:], yo[b][:], gt[b][:], ALU.mult)
        p.dma(ynT.ap()[:, t0:t0 + 128].rearrange("(c p) t -> p c t", p=128), yo[b][:])
    p.pop()
    stage_outproj(k, ynT, D, W["w_o"], h_lat, modT, 0, Tc, Tt)
    if ctx_out:
        stage_outproj(k, ynT, D, W["w_o"], h_ctx, modT, 1, 0, Tc)
```
